# Optimizing a Trainium2 kernel written in Bass

```python
import math
import jax, jax.numpy as jnp
from jax import lax
import numpy as np

D_MODEL = 1024
BATCH = 16
SEQ = 2048
DEPTH = 1

CHUNK = 64
Q_BLOCK = 128
MIX_WIDTH = D_MODEL
DIFF_WIDTH = MIX_WIDTH // 2
HGRN_WIDTH = MIX_WIDTH - DIFF_WIDTH
DIFF_HEAD_DIM = 64
DIFF_HEADS = DIFF_WIDTH // (2 * DIFF_HEAD_DIM)
DIFF_V_DIM = 2 * DIFF_HEAD_DIM
HGRN_EXPAND = 128
HGRN_HEADS = HGRN_WIDTH // HGRN_EXPAND
HGRN_DK = HGRN_EXPAND
HGRN_DV = HGRN_WIDTH // HGRN_HEADS
D_FF = ((8 * D_MODEL // 3 + 127) // 128) * 128
CONV_WIDTH = 3
EPS = 1e-6
IN_COLS = 3 * DIFF_WIDTH + 4 * HGRN_WIDTH

kernel_name = "hymba_diffattn_hgrn2_convffn"


def _rmsnorm(x, w):
    xf = x.astype(jnp.float32)
    y = xf * lax.rsqrt(jnp.mean(xf * xf, axis=-1, keepdims=True) + EPS)
    return (y * w.astype(jnp.float32)).astype(x.dtype)


def _lambda_init(layer):
    return 0.8 - 0.6 * math.exp(-0.3 * layer)


def _diff_attention(q, k, v, lam, lam_init, subln_w):
    B, S = q.shape[:2]
    nb = S // Q_BLOCK
    scale = DIFF_HEAD_DIM ** -0.5
    k_chunk = jnp.arange(S) // CHUNK
    q_blocks = jnp.moveaxis(q.reshape(B, nb, Q_BLOCK, DIFF_HEADS, 2, DIFF_HEAD_DIM), 1, 0)

    def block(args):
        qb, bi = args
        q_chunk = (bi * Q_BLOCK + jnp.arange(Q_BLOCK)) // CHUNK
        mask = k_chunk[None, :] <= q_chunk[:, None]
        s = jnp.einsum('bqhcd,bkhcd->bhcqk', qb, k).astype(jnp.float32) * scale
        p = jax.nn.softmax(jnp.where(mask, s, -jnp.inf), axis=-1)
        a = p[:, :, 0] - lam * p[:, :, 1]
        return jnp.einsum('bhqk,bkhe->bqhe', a.astype(v.dtype), v)

    o = lax.map(block, (q_blocks, jnp.arange(nb)))
    o = jnp.moveaxis(o, 0, 1).reshape(B, S, DIFF_HEADS, DIFF_V_DIM)
    o = _rmsnorm(o, subln_w) * (1.0 - lam_init)
    return o.reshape(B, S, DIFF_WIDTH)


def _hgrn2(q, f_logit, i, gate, lb, norm_w):
    B, S = q.shape[:2]
    nc = S // CHUNK
    f32 = jnp.float32
    f = lb + (1.0 - lb) * jax.nn.sigmoid(f_logit.astype(f32))
    log_f = jnp.log(f)
    k = 1.0 - f

    def to_chunks(t):
        return t.reshape(B, nc, CHUNK, t.shape[2], t.shape[3]).transpose(1, 0, 3, 2, 4)

    causal = jnp.tril(jnp.ones((CHUNK, CHUNK), bool))

    def step(state, xs):
        qc, kc, vc, gc = xs
        b = jnp.cumsum(gc, axis=-2)
        rel = jnp.where(causal[:, :, None],
                        b[:, :, :, None, :] - b[:, :, None, :, :], -jnp.inf)
        att = jnp.einsum('bhtk,bhsk,bhtsk->bhts', qc, kc, jnp.exp(rel))
        o = (jnp.einsum('bhts,bhsv->bhtv', att, vc)
             + jnp.einsum('bhtk,bhkv->bhtv', qc * jnp.exp(b), state))
        b_last = b[:, :, -1:, :]
        state = (jnp.exp(b_last[:, :, 0, :])[..., None] * state
                 + jnp.einsum('bhsk,bhsv->bhkv', kc * jnp.exp(b_last - b), vc))
        return state, o

    state0 = jnp.zeros((B, HGRN_HEADS, HGRN_DK, HGRN_DV), f32)
    _, o = lax.scan(step, state0, (to_chunks(q.astype(f32)), to_chunks(k),
                                   to_chunks(i.astype(f32)), to_chunks(log_f)))
    o = o.transpose(1, 0, 3, 2, 4).reshape(B, S, HGRN_HEADS, HGRN_DV)
    o = _rmsnorm(o, norm_w).reshape(B, S, HGRN_WIDTH)
    return (o * jax.nn.silu(gate.astype(f32))).astype(gate.dtype)


def _conv_ffn(x, w_up, conv_w, conv_b, w_down):
    S = x.shape[1]
    u, v = jnp.split(x @ w_up, 2, axis=-1)
    u_pad = jnp.pad(u, ((0, 0), (CONV_WIDTH - 1, 0), (0, 0)))
    c = conv_b
    for j in range(CONV_WIDTH):
        c = c + u_pad[:, j:j + S] * conv_w[j]
    return (jax.nn.silu(c) * v) @ w_down


def setup_inputs(seed: int = 0) -> dict:
    key = jax.random.key(seed)
    ks = jax.random.split(key, 20)
    n = jax.random.normal
    f32 = jnp.float32

    def gain(k, shape):
        return 1.0 + 0.02 * n(k, shape, f32)

    return {
        "x": n(ks[0], (BATCH, SEQ, D_MODEL), f32),
        "ln1_w": gain(ks[1], (DEPTH, D_MODEL)),
        "w_in": n(ks[2], (DEPTH, D_MODEL, IN_COLS), f32) * D_MODEL ** -0.5,
        "q_norm_w": gain(ks[3], (DEPTH, DIFF_HEAD_DIM)),
        "k_norm_w": gain(ks[4], (DEPTH, DIFF_HEAD_DIM)),
        "lam_q1": 0.1 * n(ks[5], (DEPTH, DIFF_HEAD_DIM), f32),
        "lam_k1": 0.1 * n(ks[6], (DEPTH, DIFF_HEAD_DIM), f32),
        "lam_q2": 0.1 * n(ks[7], (DEPTH, DIFF_HEAD_DIM), f32),
        "lam_k2": 0.1 * n(ks[8], (DEPTH, DIFF_HEAD_DIM), f32),
        "diff_subln_w": gain(ks[9], (DEPTH, DIFF_V_DIM)),
        "hgrn_lb_logits": 0.1 * n(ks[10], (DEPTH + 1, HGRN_WIDTH), f32),
        "hgrn_norm_w": gain(ks[11], (DEPTH, HGRN_DV)),
        "w_out": n(ks[12], (DEPTH, MIX_WIDTH, D_MODEL), f32) * MIX_WIDTH ** -0.5,
        "ln2_w": gain(ks[13], (DEPTH, D_MODEL)),
        "w_up": n(ks[14], (DEPTH, D_MODEL, 2 * D_FF), f32) * D_MODEL ** -0.5,
        "conv_w": n(ks[15], (DEPTH, CONV_WIDTH, D_FF), f32) * CONV_WIDTH ** -0.5,
        "conv_b": 0.02 * n(ks[16], (DEPTH, D_FF), f32),
        "w_down": n(ks[17], (DEPTH, D_FF, D_MODEL), f32) * D_FF ** -0.5,
    }


def reference(x, ln1_w, w_in, q_norm_w, k_norm_w, lam_q1, lam_k1, lam_q2, lam_k2,
              diff_subln_w, hgrn_lb_logits, hgrn_norm_w, w_out, ln2_w, w_up, conv_w,
              conv_b, w_down):
    B, S, _ = x.shape
    lb_all = jnp.cumsum(jax.nn.softmax(hgrn_lb_logits.astype(jnp.float32), axis=0), axis=0)
    offs = [DIFF_WIDTH, 2 * DIFF_WIDTH, 3 * DIFF_WIDTH,
            3 * DIFF_WIDTH + HGRN_WIDTH, 3 * DIFF_WIDTH + 2 * HGRN_WIDTH,
            3 * DIFF_WIDTH + 3 * HGRN_WIDTH]
    for l in range(DEPTH):
        h = _rmsnorm(x, ln1_w[l])
        proj = h @ w_in[l]
        dq, dk, dv, hq, hf, hi, hg = jnp.split(proj, offs, axis=-1)
        dq = _rmsnorm(dq.reshape(B, S, DIFF_HEADS, 2, DIFF_HEAD_DIM), q_norm_w[l])
        dk = _rmsnorm(dk.reshape(B, S, DIFF_HEADS, 2, DIFF_HEAD_DIM), k_norm_w[l])
        dv = dv.reshape(B, S, DIFF_HEADS, DIFF_V_DIM)
        lam_init = _lambda_init(l)
        lam = (jnp.exp(jnp.sum(lam_q1[l] * lam_k1[l]).astype(jnp.float32))
               - jnp.exp(jnp.sum(lam_q2[l] * lam_k2[l]).astype(jnp.float32)) + lam_init)
        o_diff = _diff_attention(dq, dk, dv, lam, lam_init, diff_subln_w[l])
        lb = lb_all[l].reshape(HGRN_HEADS, HGRN_DK)
        o_hgrn = _hgrn2(hq.reshape(B, S, HGRN_HEADS, HGRN_DK),
                        hf.reshape(B, S, HGRN_HEADS, HGRN_DK),
                        hi.reshape(B, S, HGRN_HEADS, HGRN_DV),
                        hg, lb, hgrn_norm_w[l])
        x = x + jnp.concatenate([o_diff, o_hgrn], axis=-1) @ w_out[l]
        x = x + _conv_ffn(_rmsnorm(x, ln2_w[l]), w_up[l], conv_w[l], conv_b[l], w_down[l])
    return x
```

```python
import contextlib
import math

import numpy as np
import concourse.bass as bass
import concourse.mybir as mybir
from concourse.bass_utils import run_bass_kernel_spmd

F32 = mybir.dt.float32
BF16 = mybir.dt.bfloat16
ALU = mybir.AluOpType
AF = mybir.ActivationFunctionType
AX = mybir.AxisListType

NCORES = 8
NB = 2
SEQ = 2048
D = 1024
NT = SEQ // 128
FF = 2816
NFC = FF // 128
INC = 3584
EPS = 1e-6
LAM_INIT = 0.8 - 0.6 * math.exp(-0.3 * 0)
ENGS = ("pe", "act", "dve", "pool", "sp")


class _Op:
    __slots__ = ("eng", "fn", "deps", "signals", "sig", "chan", "chan_val")


class Sched:
    def __init__(self, nc):
        self.nc = nc
        self.ops = []
        self.eng_ops = {e: [] for e in ENGS}
        self.last_writer = {}
        self.readers = {}
        self.chan_count = {}
        self.chan_last = {}
        self.eng_last = {}
        self.bar = set()

    def add(self, eng, fn, reads=(), writes=(), chan=None):
        op = _Op()
        op.eng = eng; op.fn = fn; op.chan = chan
        op.signals = False; op.sig = None; op.chan_val = None
        writes = list(writes) + [k for k in reads if isinstance(k, tuple) and k[0] == "ps"]
        deps = set(self.bar)
        for k in reads:
            w = self.last_writer.get(k)
            if w is not None:
                deps.add(w)
        for k in writes:
            w = self.last_writer.get(k)
            if w is not None:
                deps.add(w)
            for r in self.readers.get(k, ()):
                deps.add(r)
        for k in reads:
            self.readers.setdefault(k, []).append(op)
        for k in writes:
            self.last_writer[k] = op
            self.readers[k] = []
        deps.discard(op)
        if eng == "pe":
            deps = {d for d in deps if not (d.eng == "pe" and d.chan is None)}
        op.deps = deps
        if chan is not None:
            self.chan_count[chan] = self.chan_count.get(chan, 0) + 16
            op.chan_val = self.chan_count[chan]
            self.chan_last[chan] = op
        else:
            self.eng_last[eng] = op
        self.ops.append(op)
        self.eng_ops[eng].append(op)
        return op

    def barrier(self):
        self.bar = set(self.eng_last.values()) | set(self.chan_last.values())
        self.last_writer = {}
        self.readers = {}

    def emit(self, final_wait_eng="sp", limit=None):
        nc = self.nc
        if limit is not None:
            kept = self.ops[:limit]
            self.ops = kept
            ks = set(kept)
            self.eng_ops = {e: [o for o in self.eng_ops[e] if o in ks] for e in ENGS}
            self.chan_count = {}
            for o in kept:
                if o.chan is not None:
                    self.chan_count[o.chan] = max(self.chan_count.get(o.chan, 0), o.chan_val)
        for op in self.ops:
            for d in op.deps:
                if d.chan is None:
                    d.signals = True
        cnt = {e: 0 for e in ENGS}
        for op in self.ops:
            if op.chan is None and op.signals:
                cnt[op.eng] += 1
                op.sig = cnt[op.eng]
        with contextlib.ExitStack() as st:
            esem = {e: st.enter_context(nc.semaphore("s_" + e)) for e in ENGS}
            csem = {}
            for i, c in enumerate(self.chan_count):
                csem[c] = st.enter_context(nc.semaphore("c%d" % i))
            block = st.enter_context(nc.Block())
            handles = {"pe": block.tensor, "act": block.scalar, "dve": block.vector,
                       "pool": block.gpsimd, "sp": block.sync}
            for e in ENGS:
                ops = self.eng_ops[e]
                if not ops and e != final_wait_eng:
                    continue

                def body(eng, ops=ops, e=e):
                    known = {}
                    for op in ops:
                        need = {}
                        for d in op.deps:
                            if d.chan is not None:
                                key = ("c", d.chan); val = d.chan_val
                            else:
                                key = ("e", d.eng); val = d.sig
                            if val > need.get(key, 0):
                                need[key] = val
                        for key, val in need.items():
                            if known.get(key, 0) >= val:
                                continue
                            known[key] = val
                            sem = csem[key[1]] if key[0] == "c" else esem[key[1]]
                            eng.wait_ge(sem, val)
                        ins = op.fn(eng)
                        if op.chan is not None:
                            ins.then_inc(csem[op.chan], 16)
                        elif op.signals:
                            ins.then_inc(esem[e], 1)
                    if e == final_wait_eng:
                        for c, v in self.chan_count.items():
                            if known.get(("c", c), 0) < v:
                                eng.wait_ge(csem[c], v)

                handles[e](body)


class Arena:
    def __init__(self, ap_f32, nwords):
        self.ap = ap_f32
        self.n = nwords
        self.off = 0

    def alloc(self, free_shape, dtype):
        n = 1
        for s in free_shape:
            n *= s
        nbytes = n * (2 if dtype == BF16 else 4)
        nw = (nbytes + 3) // 4
        nw = (nw + 7) // 8 * 8
        assert self.off + nw <= self.n, ("arena overflow", self.off, nw, self.n)
        v = self.ap[:, self.off:self.off + nw]
        self.off += nw
        if dtype == BF16:
            v = v.bitcast(BF16)
        v = v[:, 0:n]
        if len(free_shape) == 2:
            v = v.rearrange("p (a b) -> p a b", a=free_shape[0])
        elif len(free_shape) == 3:
            v = v.rearrange("p (a b c) -> p a b c", a=free_shape[0], b=free_shape[1])
        return v


def build_program(taps=None, limit_mark=None, marks_out=None):
    nc = bass.Bass("TRN2", target_bir_lowering=False)

    def din(name, shape):
        return nc.dram_tensor(name, shape, F32, kind="ExternalInput").ap()

    x = din("x", [NB, SEQ, D])
    ln1_w = din("ln1_w", [1, D])
    w_in = din("w_in", [D, INC])
    q_norm_w = din("q_norm_w", [1, 64])
    k_norm_w = din("k_norm_w", [1, 64])
    lam_q1 = din("lam_q1", [1, 64])
    lam_k1 = din("lam_k1", [1, 64])
    lam_q2 = din("lam_q2", [1, 64])
    lam_k2 = din("lam_k2", [1, 64])
    diff_subln_w = din("diff_subln_w", [1, 128])
    hgrn_lb_logits = din("hgrn_lb_logits", [2, 512])
    hgrn_norm_w = din("hgrn_norm_w", [1, 128])
    w_out = din("w_out", [D, D])
    ln2_w = din("ln2_w", [1, D])
    w_up = din("w_up", [D, 2 * FF])
    conv_w = din("conv_w", [3, FF])
    conv_b = din("conv_b", [1, FF])
    w_down = din("w_down", [FF, D])
    out = nc.dram_tensor("out", [NB, SEQ, D], F32, kind="ExternalOutput").ap()
    tap_t = {}
    if taps:
        for name, (shape, dt_) in taps.items():
            tap_t[name] = nc.dram_tensor("tap_" + name, shape, dt_, kind="ExternalOutput").ap()

    es = contextlib.ExitStack()
    with es:
        def sb(name, shape, dt_):
            return es.enter_context(nc.sbuf_tensor(name, shape, dt_))

        identf = sb("identf", [128, 128], F32)
        ident = sb("ident", [128, 128], BF16)
        blockones = sb("blockones", [128, 128], BF16)
        mask2 = sb("mask2", [128, 64], F32)
        onescol = sb("onescol", [128, 1], F32)
        NSTG = 116
        stage = sb("stage", [128, 128], F32)
        cst = sb("cst", [128, NSTG], F32)
        ln1c = cst[:, 0:8]
        ln2c = cst[:, 8:16]
        cw = cst[:, 16:82].rearrange("p (j c) -> p j c", j=3)
        cb = cst[:, 82:104]
        lbl = cst[:, 104:112].rearrange("p (r h) -> p r h", r=2)
        hgw = cst[:, 115:116]
        gqk = sb("gqk", [128, 2], F32)
        subw = sb("subw", [128, 1], F32)
        lamv = sb("lamv", [128, 4, 64], F32)
        lamp = sb("lamp", [128, 2, 64], F32)
        lams = sb("lams", [128, 2], F32)
        neglam = sb("neglam", [128, 1], F32)
        lbt = sb("lbt", [128, 4], F32)
        lb = sb("lb", [128, 4], F32)
        ln1mlb = sb("ln1mlb", [128, 4], F32)
        rstd2 = sb("rstd2", [128, NB * NT], F32)
        st4 = sb("st4", [128, 3, 8], F32)
        carry = sb("carry", [128, 4], F32)

        nwords = nc.sbuf_bytes_remaining // 4 - 64
        arena_t = sb("arena", [128, nwords], F32)
        ps_t = es.enter_context(nc.psum_tensor("ps", [128, 8, 512], F32))

        def ps(bank):
            return ps_t[:, bank, :]

        def psb(bank):
            return ps_t[:, bank, :].bitcast(BF16)

        S = Sched(nc)
        add = S.add
        cnt = {}
        marks = {}

        def mark(name):
            marks[name] = len(S.ops)

        def rot(name, n):
            v = cnt.get(name, 0)
            cnt[name] = v + 1
            return v % n

        def tap(name, src_ap, reads):
            if name in tap_t:
                add("sp", lambda e: e.dma_start(out=tap_t[name], in_=src_ap), reads=reads,
                    chan=("tap", name))

        def ld_small(dst, src, key):
            add("sp", lambda e: e.dma_start(out=dst, in_=src), writes=[key], chan=("c", key))

        add("pool", lambda e: e.memset(stage[:, :], 0.0), writes=["stage0"])
        srows = [
            (0, 8, ln1_w.rearrange("o (k p) -> (o k) p", p=128)),
            (8, 8, ln2_w.rearrange("o (k p) -> (o k) p", p=128)),
            (16, 66, conv_w.rearrange("j (c p) -> (j c) p", p=128)),
            (82, 22, conv_b.rearrange("o (c p) -> (o c) p", p=128)),
            (104, 8, hgrn_lb_logits.rearrange("r (h k) -> (r h) k", k=128)),
            (114, 1, diff_subln_w),
            (115, 1, hgrn_norm_w),
        ]
        skeys = ["stage0"]
        for r0_, n_, src_ in srows:
            add("sp", lambda e, r0_=r0_, n_=n_, src_=src_: e.dma_start(out=stage[r0_:r0_ + n_, :], in_=src_),
                reads=["stage0"], writes=[("stage", r0_)], chan=("c", "stage", r0_))
            skeys.append(("stage", r0_))
        for c in range(2):
            for r0_, src_ in ((112, q_norm_w), (113, k_norm_w)):
                add("sp", lambda e, r0_=r0_, src_=src_, c=c: e.dma_start(
                    out=stage[r0_:r0_ + 1, c * 64:(c + 1) * 64], in_=src_),
                    reads=["stage0"], writes=[("stage", r0_, c)], chan=("c", "stage", r0_, c))
                skeys.append(("stage", r0_, c))
        for i, lv in enumerate((lam_q1, lam_k1, lam_q2, lam_k2)):
            ld_small(lamv[:, i, :], lv.partition_broadcast(128), ("lamv", i))

        add("pool", lambda e: e.memset(identf[:, :], 1.0), writes=["identf"])
        add("pool", lambda e: e.affine_select(out=identf[:, :], in_=identf[:, :], pattern=[[-1, 128]],
                                              compare_op=ALU.is_equal, fill=0.0, base=0,
                                              channel_multiplier=1),
            reads=["identf"], writes=["identf"])
        add("dve", lambda e: e.tensor_copy(ident[:, :], identf[:, :]), reads=["identf"], writes=["ident"])
        add("pe", lambda e: e.transpose(ps_t[:, 0, 0:NSTG], stage[0:NSTG, :], identf[0:NSTG, 0:NSTG]),
            reads=skeys + ["identf"], writes=[("ps", 0)])
        add("dve", lambda e: e.tensor_copy(cst[:, :], ps_t[:, 0, 0:NSTG]), reads=[("ps", 0)],
            writes=["ln1c", "ln2c", "cw", "cb", "lbl", "cst"])
        add("pool", lambda e: e.memset(mask2[:, :], 1.0), writes=["mask2"])
        for hf_ in range(2):
            add("pool", lambda e, hf_=hf_: e.affine_select(
                out=mask2[hf_ * 64:(hf_ + 1) * 64, :], in_=mask2[hf_ * 64:(hf_ + 1) * 64, :],
                pattern=[[1, 64]], compare_op=ALU.is_ge, fill=0.0, base=0, channel_multiplier=-1),
                reads=["mask2"], writes=["mask2"])
        add("dve", lambda e: e.memset(blockones[:, :], 0.0), writes=["blockones"])
        for hf_ in range(2):
            add("dve", lambda e, hf_=hf_: e.memset(
                blockones[hf_ * 64:(hf_ + 1) * 64, hf_ * 64:(hf_ + 1) * 64], 1.0 / 64.0),
                writes=["blockones"])
        add("pool", lambda e: e.memset(onescol[:, :], 1.0), writes=["onescol"])
        add("dve", lambda e: e.tensor_scalar(gqk[:, 0:1], cst[:, 112:113], 0.125, None, ALU.mult),
            reads=["cst"], writes=["gqk"])
        add("dve", lambda e: e.tensor_copy(gqk[:, 1:2], cst[:, 113:114]), reads=["cst"], writes=["gqk"])
        add("dve", lambda e: e.tensor_scalar(subw[:, :], cst[:, 114:115], 1.0 - LAM_INIT, None, ALU.mult),
            reads=["cst"], writes=["subw"])
        add("dve", lambda e: e.tensor_tensor(lamp[:, 0, :], lamv[:, 0, :], lamv[:, 1, :], ALU.mult),
            reads=[("lamv", 0), ("lamv", 1)], writes=[("lamp", 0)])
        add("dve", lambda e: e.tensor_tensor(lamp[:, 1, :], lamv[:, 2, :], lamv[:, 3, :], ALU.mult),
            reads=[("lamv", 2), ("lamv", 3)], writes=[("lamp", 1)])
        add("dve", lambda e: e.reduce_sum(lams[:, :], lamp[:, :, :], AX.X),
            reads=[("lamp", 0), ("lamp", 1)], writes=["lams"])
        add("act", lambda e: e.activation(lams[:, :], lams[:, :], AF.Exp), reads=["lams"], writes=["lams"])
        add("dve", lambda e: e.tensor_tensor(neglam[:, :], lams[:, 1:2], lams[:, 0:1], ALU.subtract),
            reads=["lams"], writes=["neglam"])
        add("dve", lambda e: e.tensor_scalar(neglam[:, :], neglam[:, :], -LAM_INIT, None, ALU.add),
            reads=["neglam"], writes=["neglam"])
        add("dve", lambda e: e.tensor_tensor(lbt[:, :], lbl[:, 1, :], lbl[:, 0, :], ALU.subtract),
            reads=["lbl"], writes=["lbt"])
        add("act", lambda e: e.activation(lbt[:, :], lbt[:, :], AF.Exp), reads=["lbt"], writes=["lbt"])
        add("dve", lambda e: e.tensor_scalar(lbt[:, :], lbt[:, :], 1.0, None, ALU.add),
            reads=["lbt"], writes=["lbt"])
        add("dve", lambda e: e.reciprocal(lb[:, :], lbt[:, :]), reads=["lbt"], writes=["lb"])
        add("act", lambda e: e.activation(ln1mlb[:, :], lb[:, :], AF.Ln, bias=1.0, scale=-1.0),
            reads=["lb"], writes=["ln1mlb"])

        mark("consts")
        A1 = Arena(arena_t[:, :], nwords)
        wout = A1.alloc([8, D], BF16)
        winb = [A1.alloc([8, 512], BF16) for _ in range(4)]
        hT = A1.alloc([8, SEQ], BF16)
        ocatT = A1.alloc([8, SEQ], BF16)
        xt = [A1.alloc([D], F32) for _ in range(3)]
        junk = A1.alloc([D], BF16)
        hn = [A1.alloc([D], BF16) for _ in range(2)]
        x1t = [A1.alloc([D], F32) for _ in range(2)]
        local_base = A1.off
        qkT = [A1.alloc([2, SEQ], BF16) for _ in range(2)]
        vaug = A1.alloc([NT, 2, 130], BF16)
        pt = [A1.alloc([2, 256], BF16) for _ in range(3)]
        pd = [A1.alloc([2, 256], BF16) for _ in range(2)]
        sq = [A1.alloc([512], BF16) for _ in range(2)]
        lnms = [A1.alloc([512], F32) for _ in range(2)]
        t0 = [A1.alloc([2, 128], F32) for _ in range(2)]
        t1a = [A1.alloc([2, 128], F32) for _ in range(2)]
        od = [A1.alloc([2, 128], F32) for _ in range(2)]
        odn = [A1.alloc([2, 128], BF16) for _ in range(2)]
        junk2 = A1.alloc([128], BF16)
        rz = [A1.alloc([4], F32) for _ in range(2)]
        rzl = [A1.alloc([2], F32) for _ in range(2)]
        attn_end = A1.off
        A1.off = local_base
        he = [A1.alloc([256], F32) for _ in range(2)]
        hL1 = [A1.alloc([256], F32) for _ in range(2)]
        hL2 = [A1.alloc([256], F32) for _ in range(2)]
        ht1 = [A1.alloc([256], F32) for _ in range(2)]
        heg = [A1.alloc([256], F32) for _ in range(2)]
        hBx = [A1.alloc([264], F32) for _ in range(2)]
        QtT = [A1.alloc([4, 256], BF16) for _ in range(2)]
        KtT = [A1.alloc([4, 256], BF16) for _ in range(2)]
        Gt = [A1.alloc([4, 256], BF16) for _ in range(2)]
        vtok = [A1.alloc([2, 512], BF16) for _ in range(2)]
        gdec = [A1.alloc([4, 4], F32) for _ in range(2)]
        Ktok = [A1.alloc([512], BF16) for _ in range(2)]
        AT = [A1.alloc([4, 64], BF16) for _ in range(2)]
        Ub = A1.alloc([4, 128], F32)
        Sst = [A1.alloc([4, 128], F32) for _ in range(2)]
        Sbf = [A1.alloc([4, 128], BF16) for _ in range(2)]
        osq = A1.alloc([4, 128], F32)
        onb = A1.alloc([4, 128], BF16)
        ssD = A1.alloc([3, 4], F32)
        A1.off = max(A1.off, attn_end)

        A2 = Arena(arena_t[:, :], nwords)
        wup = A2.alloc([8, 2 * FF], BF16)
        wdown = A2.alloc([NFC, D], BF16)
        x1p = [A2.alloc([2, D], F32) for _ in range(2)]
        h2n = [A2.alloc([2, D], BF16) for _ in range(2)]
        h2T = [A2.alloc([8, 256], BF16) for _ in range(2)]
        accb = [A2.alloc([256], F32) for _ in range(3)]
        slb = [A2.alloc([256], F32) for _ in range(2)]
        gbuf = [A2.alloc([256], BF16) for _ in range(4)]
        halo = A2.alloc([NFC, 2], F32)

        add("pool", lambda e: e.dma_start(out=wout, in_=w_out.rearrange("(k p) n -> p k n", p=128)),
            writes=["wout"], chan="wout")

        def load_win(g, slot):
            add("pool", lambda e: e.dma_start(
                out=winb[slot], in_=w_in[:, g * 512:(g + 1) * 512].rearrange("(k p) n -> p k n", p=128)),
                writes=[("win", slot)], chan=("win", slot))

        def stat(i):
            return st4[:, 0, i:i + 1], st4[:, 1, i:i + 1], st4[:, 2, i:i + 1]

        for b in range(NB):
            for g in range(4):
                load_win(g, g)
            for i in range(NT):
                xs = rot("xt", 3)
                add("sp", lambda e, xs=xs, i=i, b=b: e.dma_start(out=xt[xs], in_=x[b, i * 128:(i + 1) * 128, :]),
                    writes=[("xt", xs)], chan=("xt", xs))
                si = rot("st", 8)
                ss, ln_, rs = stat(si)
                add("act", lambda e, xs=xs, ss=ss: e.activation(junk, xt[xs], AF.Square, accum_out=ss),
                    reads=[("xt", xs)], writes=["junk", ("ss", si)])
                add("act", lambda e, ss=ss, ln_=ln_: e.activation(ln_, ss, AF.Ln, bias=EPS, scale=1.0 / D),
                    reads=[("ss", si)], writes=[("ln", si)])
                add("act", lambda e, rs=rs, ln_=ln_: e.activation(rs, ln_, AF.Exp, scale=-0.5),
                    reads=[("ln", si)], writes=[("rs", si)])
                hs = rot("hn", 2)
                add("dve", lambda e, hs=hs, xs=xs, rs=rs: e.tensor_scalar(hn[hs], xt[xs], rs, None, ALU.mult),
                    reads=[("xt", xs), ("rs", si)], writes=[("hn", hs)])
                tb = (2, 7)[rot("tpA", 2)]

                def tr8(e, hs=hs, tb=tb):
                    ins = None
                    for kc in range(8):
                        ins = e.transpose(psb(tb)[:, kc * 128:(kc + 1) * 128],
                                          hn[hs][:, kc * 128:(kc + 1) * 128], ident[:, :])
                    return ins
                add("pe", tr8, reads=[("hn", hs), "ident"], writes=[("ps", tb)])
                add("dve", lambda e, tb=tb, i=i: e.tensor_tensor(
                    hT[:, :, i * 128:(i + 1) * 128],
                    psb(tb)[:, 0:1024].rearrange("p (k t) -> p k t", k=8),
                    ln1c[:, :].unsqueeze(2).to_broadcast([128, 8, 128]), ALU.mult),
                    reads=[("ps", tb), "ln1c"], writes=[("hT", i)])
            mark("A%d" % b)
            if b == 0:
                tap("hT", hT, [("hT", i) for i in range(NT)])

            for hp in range(2):
                S.barrier()
                if hp == 0:
                    add("dve", lambda e: e.memset(vaug[:, :, :, 128:129], 1.0), writes=["vaug_ones"])
                    add("dve", lambda e: e.memset(vaug[:, :, :, 129:130], 0.0), writes=["vaug_ones"])
                    for k_ in range(2):
                        add("dve", lambda e, k_=k_: e.memset(pd[k_][64:128, :, 0:64], 0.0),
                            writes=[("pdz", k_)])
                items = [(T, hh, which) for T in range(4) for hh in range(2) for which in range(2)]
                st_ = {}

                def b_front(k):
                    T, hh, which = items[k]
                    h = 2 * hp + hh
                    pb = (0, 1, 5, 6)[rot("pa", 4)]
                    j = rot("sq", 2)
                    st_[k] = (pb, j)

                    def proj(e, which=which, h=h, T=T, pb=pb):
                        ins = None
                        for kc in range(8):
                            ins = e.matmul(ps(pb), winb[which][:, kc, h * 128:(h + 1) * 128],
                                           hT[:, kc, T * 512:(T + 1) * 512],
                                           start=(kc == 0), stop=(kc == 7))
                        return ins
                    add("pe", proj, reads=[("win", which)] + [("hT", 4 * T + jj) for jj in range(4)],
                        writes=[("ps", pb)])
                    add("act", lambda e, j=j, pb=pb: e.activation(sq[j], ps(pb), AF.Square),
                        reads=[("ps", pb)], writes=[("sq", j)])

                def b_back(k):
                    T, hh, which = items[k]
                    pb, j = st_[k]
                    mb = (3, 4)[rot("ms", 2)]
                    add("pe", lambda e, j=j, mb=mb: e.matmul(ps(mb), blockones[:, :], sq[j],
                                                             start=True, stop=True),
                        reads=[("sq", j), "blockones"], writes=[("ps", mb)])
                    add("act", lambda e, j=j, mb=mb: e.activation(lnms[j], ps(mb), AF.Ln, bias=EPS),
                        reads=[("ps", mb)], writes=[("lnms", j)])
                    add("act", lambda e, j=j: e.activation(lnms[j], lnms[j], AF.Exp, scale=-0.5),
                        reads=[("lnms", j)], writes=[("lnms", j)])
                    add("dve", lambda e, j=j, pb=pb, which=which, hh=hh, T=T: e.scalar_tensor_tensor(
                        qkT[which][:, hh, T * 512:(T + 1) * 512], ps(pb), gqk[:, which:which + 1],
                        lnms[j], ALU.mult, ALU.mult),
                        reads=[("ps", pb), ("lnms", j), "gqk"], writes=[("qkT", which, hh, T)])

                for k in range(len(items)):
                    b_front(k)
                    if k >= 1:
                        b_back(k - 1)
                b_back(len(items) - 1)
                for i in range(NT):
                    pb = (0, 1, 5, 6)[rot("pa", 4)]

                    def projv(e, i=i, pb=pb, hp=hp):
                        ins = None
                        for kc in range(8):
                            ins = e.matmul(ps(pb)[:, 0:256], hT[:, kc, i * 128:(i + 1) * 128],
                                           winb[2][:, kc, hp * 256:(hp + 1) * 256],
                                           start=(kc == 0), stop=(kc == 7))
                        return ins
                    add("pe", projv, reads=[("win", 2), ("hT", i)], writes=[("ps", pb)])
                    add("act", lambda e, i=i, pb=pb: e.activation(
                        vaug[:, i, :, 0:128], ps(pb)[:, 0:256].rearrange("p (h e) -> p h e", h=2), AF.Copy),
                        reads=[("ps", pb)], writes=[("vaug", i)])
                mark("B%d_%d" % (b, hp))
                if b == 0 and hp == 0:
                    tap("qT", qkT[0], [("qkT", 0, hh, T) for hh in range(2) for T in range(4)])
                    tap("kT", qkT[1], [("qkT", 1, hh, T) for hh in range(2) for T in range(4)])
                    tap("vaug", vaug, [("vaug", i) for i in range(NT)] + ["vaug_ones"])
                if hp == 1:
                    for g, slot in ((4, 0), (5, 1), (6, 2)):
                        load_win(g, slot)

                pending = []
                for hh in range(2):
                    h = 2 * hp + hh
                    for u in range(8):
                        ab0 = 5
                        ns = 2 * u + 2
                        info = {}

                        def emit_qk(i, hh=hh, u=u, info=info):
                            scb = (0, 3)[rot("sc", 2)]
                            lo = 128 if i == 2 * u + 1 else 0
                            n = 256 - lo
                            info[i] = (scb, lo, n)

                            def qk(e, hh=hh, u=u, i=i, scb=scb, lo=lo, n=n):
                                ins = None
                                for c in range(2):
                                    ins = e.matmul(ps(scb + c)[:, 0:n],
                                                   qkT[1][c * 64:(c + 1) * 64, hh, i * 128:(i + 1) * 128],
                                                   qkT[0][c * 64:(c + 1) * 64, hh, u * 256 + lo:(u + 1) * 256],
                                                   start=True, stop=True)
                                return ins
                            add("pe", qk, reads=[("qkT", 1, hh, i // 4), ("qkT", 0, hh, u // 2)],
                                writes=[("ps", scb), ("ps", scb + 1)])

                        def emit_exp_pv(i, hh=hh, u=u, info=info, ab0=ab0):
                            scb, lo, n = info[i]
                            scv = ps_t[:, scb:scb + 2, 0:256]
                            if i < 2 * u:
                                pi = rot("pt", 3)
                                pbuf = pt[pi]; pkey = ("pt", pi)
                                add("act", lambda e, pbuf=pbuf, scv=scv: e.activation(pbuf, scv, AF.Exp),
                                    reads=[("ps", scb), ("ps", scb + 1)], writes=[pkey])
                                extra = []
                            else:
                                pi = rot("pd", 2)
                                pbuf = pd[pi]; pkey = ("pd", pi)
                                add("act", lambda e, pbuf=pbuf, scv=scv, n=n: e.activation(
                                    pbuf[0:64, :, 0:n], scv[0:64, :, 0:n], AF.Exp),
                                    reads=[("ps", scb), ("ps", scb + 1)], writes=[pkey])
                                add("act", lambda e, pbuf=pbuf, scv=scv, n=n: e.activation(
                                    pbuf[64:128, :, 64:n], scv[64:128, :, 64:n], AF.Exp),
                                    reads=[("ps", scb), ("ps", scb + 1)], writes=[pkey])
                                extra = [("pdz", pi)]
                            return pbuf, pkey, extra

                        def emit_pv(i, pbuf, pkey, extra, hh=hh, u=u, ab0=ab0):
                            jbs = (1,) if i == 2 * u + 1 else (0, 1)

                            def pv(e, pbuf=pbuf, i=i, u=u, hh=hh, jbs=jbs, ab0=ab0):
                                ins = None
                                for jb in jbs:
                                    off = 0 if i == 2 * u + 1 else jb * 128
                                    for c in range(2):
                                        ins = e.matmul(ps(ab0 + jb)[:, c * 256:c * 256 + 130],
                                                       pbuf[:, c, off:off + 128], vaug[:, i, hh, :],
                                                       start=(i == 0 and c == 0), stop=(i == 2 * u + jb),
                                                       skip_group_check=True)
                                return ins
                            add("pe", pv, reads=[pkey, ("vaug", i), "vaug_ones"] + extra,
                                writes=[("ps", ab0 + jb) for jb in jbs])

                        emit_qk(0)
                        for i in range(ns):
                            pbuf, pkey, extra = emit_exp_pv(i)
                            if i + 1 < ns:
                                emit_qk(i + 1)
                            emit_pv(i, pbuf, pkey, extra)
                            if i == 0 and pending:
                                pending.pop(0)()
                        k_ = rot("fin", 2)
                        accv = ps_t[:, ab0:ab0 + 2, :].rearrange("p j (c w) -> p j c w", c=2)
                        rzv = rz[k_].rearrange("p (j c) -> p j c", j=2)
                        add("dve", lambda e, accv=accv, rzv=rzv: e.reciprocal(rzv, accv[:, :, :, 128]),
                            reads=[("ps", ab0), ("ps", ab0 + 1)], writes=[("rz", k_)])
                        add("dve", lambda e, rzv=rzv, k_=k_: e.tensor_scalar(
                            rzl[k_], rzv[:, :, 1], neglam[:, 0:1], None, ALU.mult),
                            reads=[("rz", k_), "neglam"], writes=[("rzl", k_)])
                        add("dve", lambda e, accv=accv, rzv=rzv, k_=k_: e.tensor_tensor(
                            t0[k_], accv[:, :, 0, 0:128], rzv[:, :, 0:1].to_broadcast([128, 2, 128]), ALU.mult),
                            reads=[("ps", ab0), ("ps", ab0 + 1), ("rz", k_)], writes=[("t0", k_)])
                        add("dve", lambda e, accv=accv, k_=k_: e.tensor_tensor(
                            t1a[k_], accv[:, :, 1, 0:128],
                            rzl[k_].unsqueeze(2).to_broadcast([128, 2, 128]), ALU.mult),
                            reads=[("ps", ab0), ("ps", ab0 + 1), ("rzl", k_)], writes=[("t1a", k_)])
                        add("pool", lambda e, k_=k_: e.tensor_tensor(od[k_], t0[k_], t1a[k_], ALU.add),
                            reads=[("t0", k_), ("t1a", k_)], writes=[("od", k_)])
                        si = rot("st", 8)
                        si2 = rot("st", 8)
                        assert si2 == si + 1
                        ssw = st4[:, 0, si:si + 2]; lnw = st4[:, 1, si:si + 2]; rsw = st4[:, 2, si:si + 2]
                        for jb in range(2):
                            add("act", lambda e, k_=k_, jb=jb, si=si: e.activation(
                                junk2, od[k_][:, jb, :], AF.Square, accum_out=st4[:, 0, si + jb:si + jb + 1]),
                                reads=[("od", k_)], writes=["junk2", ("ss", si + jb)])
                        add("act", lambda e, ssw=ssw, lnw=lnw: e.activation(lnw, ssw, AF.Ln, bias=EPS, scale=1.0 / 128),
                            reads=[("ss", si), ("ss", si + 1)], writes=[("ln", si), ("ln", si + 1)])
                        add("act", lambda e, rsw=rsw, lnw=lnw: e.activation(rsw, lnw, AF.Exp, scale=-0.5),
                            reads=[("ln", si), ("ln", si + 1)], writes=[("rs", si), ("rs", si + 1)])
                        add("dve", lambda e, k_=k_, rsw=rsw: e.tensor_tensor(
                            odn[k_], od[k_], rsw.unsqueeze(2).to_broadcast([128, 2, 128]), ALU.mult),
                            reads=[("od", k_), ("rs", si), ("rs", si + 1)], writes=[("odn", k_)])

                        def fin_tail(k_=k_, h=h, u=u, b=b, hp=hp, hh=hh):
                            tb = (2, 7)[rot("tpC", 2)]

                            def tr2(e, k_=k_, tb=tb):
                                ins = None
                                for jb in range(2):
                                    ins = e.transpose(psb(tb)[:, jb * 128:(jb + 1) * 128], odn[k_][:, jb, :], ident[:, :])
                                return ins
                            add("pe", tr2, reads=[("odn", k_), "ident"], writes=[("ps", tb)])
                            add("act", lambda e, tb=tb, h=h, u=u: e.activation(
                                ocatT[:, h, u * 256:(u + 1) * 256], psb(tb)[:, 0:256], AF.Identity, scale=subw[:, 0:1]),
                                reads=[("ps", tb), "subw"], writes=[("ocatT", h, u)])
                        pending.append(fin_tail)
                        mark("Cu_%d_%d_%d_%d" % (b, hp, hh, u))
                while pending:
                    pending.pop(0)()

            mark("C%d" % b)
            S.barrier()
            SL_HQ, SL_HF, SL_HI, SL_HG = 3, 0, 1, 2
            dpend = []
            for tt in range(8):
                par = tt % 2
                tok0 = tt * 256
                hkeys = [("hT", 2 * tt), ("hT", 2 * tt + 1)]
                for bi in range(2):
                    i = 2 * tt + bi
                    pb = (0, 1, 3)[rot("pd3", 3)]

                    def projhi(e, i=i, pb=pb):
                        ins = None
                        for kc in range(8):
                            ins = e.matmul(ps(pb), hT[:, kc, i * 128:(i + 1) * 128], winb[SL_HI][:, kc, :],
                                           start=(kc == 0), stop=(kc == 7))
                        return ins
                    add("pe", projhi, reads=[("win", SL_HI), ("hT", i)], writes=[("ps", pb)])
                    add("act", lambda e, par=par, bi=bi, pb=pb: e.activation(vtok[par][:, bi, :], ps(pb), AF.Copy),
                        reads=[("ps", pb)], writes=[("vtok", par, bi)])
                for h in range(4):
                    s_ = h % 2

                    def projf(slot, pb, h=h, tok0=tok0):
                        def f(e):
                            ins = None
                            for kc in range(8):
                                ins = e.matmul(ps(pb)[:, 0:256], winb[slot][:, kc, h * 128:(h + 1) * 128],
                                               hT[:, kc, tok0:tok0 + 256], start=(kc == 0), stop=(kc == 7))
                            return ins
                        return f
                    pb = (0, 1, 3)[rot("pd3", 3)]
                    add("pe", projf(SL_HF, pb), reads=[("win", SL_HF)] + hkeys, writes=[("ps", pb)])
                    add("act", lambda e, s_=s_, pb=pb: e.activation(he[s_], ps(pb)[:, 0:256], AF.Exp, scale=-1.0),
                        reads=[("ps", pb)], writes=[("he", s_)])
                    add("act", lambda e, s_=s_: e.activation(hL1[s_], he[s_], AF.Ln, bias=1.0),
                        reads=[("he", s_)], writes=[("hL1", s_)])
                    add("act", lambda e, s_=s_, h=h: e.activation(hL2[s_], he[s_], AF.Ln, bias=1.0, scale=lb[:, h:h + 1]),
                        reads=[("he", s_), "lb"], writes=[("hL2", s_)])
                    add("dve", lambda e, s_=s_, pb=pb: e.tensor_tensor(ht1[s_], ps(pb)[:, 0:256], hL1[s_], ALU.add),
                        reads=[("ps", pb), ("hL1", s_)], writes=[("ht1", s_)])
                    add("pool", lambda e, s_=s_: e.tensor_tensor(hL2[s_], hL2[s_], hL1[s_], ALU.subtract),
                        reads=[("hL2", s_), ("hL1", s_)], writes=[("hL2", s_)])
                    if tt == 0:
                        add("pool", lambda e, s_=s_: e.memset(hBx[s_][:, 0:1], 0.0), writes=[("hBx", s_)])
                    else:
                        add("pool", lambda e, s_=s_, h=h: e.tensor_copy(hBx[s_][:, 0:1], carry[:, h:h + 1]),
                            reads=[("carry", h)], writes=[("hBx", s_)])
                    add("dve", lambda e, s_=s_: e.tensor_tensor_scan(
                        hBx[s_][:, 1:257], onescol[:, 0:1].to_broadcast([128, 256]), hL2[s_],
                        hBx[s_][:, 0:1], ALU.mult, ALU.add),
                        reads=[("hL2", s_), ("hBx", s_), "onescol"], writes=[("hBx", s_)])
                    add("pool", lambda e, s_=s_, h=h: e.tensor_copy(carry[:, h:h + 1], hBx[s_][:, 256:257]),
                        reads=[("hBx", s_)], writes=[("carry", h)])
                    add("act", lambda e, s_=s_, h=h: e.activation(ht1[s_], ht1[s_], AF.Exp, scale=-1.0,
                                                                  bias=ln1mlb[:, h:h + 1]),
                        reads=[("ht1", s_), "ln1mlb"], writes=[("ht1", s_)])
                    add("dve", lambda e, s_=s_: e.tensor_tensor(
                        he[s_].rearrange("p (c t) -> p c t", c=4),
                        hBx[s_][:, 1:257].rearrange("p (c t) -> p c t", c=4),
                        hBx[s_][:, 0:256:64].unsqueeze(2).to_broadcast([128, 4, 64]), ALU.subtract),
                        reads=[("hBx", s_), ("he", s_), ("hL2", s_)], writes=[("he", s_)])
                    add("act", lambda e, s_=s_: e.activation(hL1[s_], he[s_], AF.Exp),
                        reads=[("he", s_), ("ht1", s_)], writes=[("hL1", s_)])
                    add("act", lambda e, s_=s_: e.activation(he[s_], he[s_], AF.Exp, scale=-1.0),
                        reads=[("he", s_), ("hL1", s_)], writes=[("he", s_)])
                    pb2 = (0, 1, 3)[rot("pd3", 3)]
                    add("pe", projf(SL_HQ, pb2), reads=[("win", SL_HQ)] + hkeys, writes=[("ps", pb2)])
                    add("dve", lambda e, s_=s_, pb2=pb2, par=par, h=h: e.tensor_tensor(
                        QtT[par][:, h, :], ps(pb2)[:, 0:256], hL1[s_], ALU.mult),
                        reads=[("ps", pb2), ("hL1", s_)], writes=[("QtT", par, h)])
                    add("pool", lambda e, s_=s_, par=par, h=h: e.tensor_tensor(
                        KtT[par][:, h, :], ht1[s_], he[s_], ALU.mult),
                        reads=[("ht1", s_), ("he", s_)], writes=[("KtT", par, h)])
                    add("pool", lambda e, s_=s_, par=par, h=h: e.tensor_copy(
                        gdec[par][:, h, :], hL1[s_].rearrange("p (c t) -> p c t", c=4)[:, :, 63]),
                        reads=[("hL1", s_)], writes=[("gdec", par, h)])
                    pb3 = (0, 1, 3)[rot("pd3", 3)]
                    add("pe", projf(SL_HG, pb3), reads=[("win", SL_HG)] + hkeys, writes=[("ps", pb3)])
                    add("act", lambda e, s_=s_, pb3=pb3: e.activation(heg[s_], ps(pb3)[:, 0:256], AF.Exp, scale=-1.0),
                        reads=[("ps", pb3)], writes=[("heg", s_)])
                    add("act", lambda e, s_=s_: e.activation(heg[s_], heg[s_], AF.Ln, bias=1.0),
                        reads=[("heg", s_)], writes=[("heg", s_)])
                    add("act", lambda e, s_=s_: e.activation(heg[s_], heg[s_], AF.Exp, scale=-1.0),
                        reads=[("heg", s_)], writes=[("heg", s_)])
                    add("dve", lambda e, s_=s_, pb3=pb3, par=par, h=h: e.tensor_tensor(
                        Gt[par][:, h, :], ps(pb3)[:, 0:256], heg[s_], ALU.mult),
                        reads=[("ps", pb3), ("heg", s_)], writes=[("Gt", par, h)])
                if b == 0 and tt == 0:
                    tap("QtT", QtT[0], [("QtT", 0, h) for h in range(4)])
                    tap("KtT", KtT[0], [("KtT", 0, h) for h in range(4)])
                    tap("Gt", Gt[0], [("Gt", 0, h) for h in range(4)])
                for bi in range(2):
                    kt = rot("ktok", 2)

                    def trk(e, par=par, bi=bi):
                        ins = None
                        for h in range(4):
                            ins = e.transpose(psb(7)[:, 512 + h * 128:512 + (h + 1) * 128],
                                              KtT[par][:, h, bi * 128:(bi + 1) * 128], ident[:, :])
                        return ins
                    add("pe", trk, reads=[("KtT", par, h) for h in range(4)] + ["ident"], writes=[("ps", 7)])
                    add("act", lambda e, kt=kt: e.activation(Ktok[kt], psb(7)[:, 512:1024], AF.Copy),
                        reads=[("ps", 7)], writes=[("Ktok", kt)])
                    for ce in range(2):
                        c = 2 * bi + ce
                        cg = 4 * tt + c
                        r0, r1 = ce * 64, ce * 64 + 64
                        tc0 = c * 64
                        ah = rot("A", 2)
                        at = rot("AT", 2)

                        def amm(e, par=par, tc0=tc0, r0=r0, r1=r1, ah=ah):
                            ins = None
                            for h in range(4):
                                ins = e.matmul(ps_t[r0:r1, 4, ah * 256 + h * 64:ah * 256 + (h + 1) * 64],
                                               KtT[par][:, h, tc0:tc0 + 64], QtT[par][:, h, tc0:tc0 + 64],
                                               start=True, stop=True)
                            return ins
                        add("pe", amm, reads=[("KtT", par, h) for h in range(4)] + [("QtT", par, h) for h in range(4)],
                            writes=[("ps", 4)])
                        add("dve", lambda e, r0=r0, r1=r1, ah=ah, at=at: e.tensor_tensor(
                            AT[at][r0:r1, :, :],
                            ps_t[r0:r1, 4, ah * 256:(ah + 1) * 256].rearrange("p (h t) -> p h t", h=4),
                            mask2[r0:r1, :].unsqueeze(1).to_broadcast([64, 4, 64]), ALU.mult),
                            reads=[("ps", 4), "mask2"], writes=[("AT", at, ce)])
                        so = cnt.get("S", 0) % 2
                        ob = (5, 2)[cg % 2]

                        def omm(e, par=par, bi=bi, tc0=tc0, r0=r0, r1=r1, at=at, cg=cg, so=so, ob=ob):
                            ins = None
                            for h in range(4):
                                ins = e.matmul(ps_t[r0:r1, ob, h * 128:(h + 1) * 128], AT[at][r0:r1, h, :],
                                               vtok[par][r0:r1, bi, h * 128:(h + 1) * 128],
                                               start=True, stop=(cg == 0))
                                if cg > 0:
                                    ins = e.matmul(ps_t[r0:r1, ob, h * 128:(h + 1) * 128],
                                                   QtT[par][:, h, tc0:tc0 + 64], Sbf[so][:, h, :],
                                                   start=False, stop=True)
                            return ins
                        add("pe", omm, reads=[("AT", at, ce), ("vtok", par, bi), ("Sbf", so)] +
                            [("QtT", par, h) for h in range(4)], writes=[("ps", ob)])
                        last = (cg == 31)
                        flush_now = list(dpend)
                        del dpend[:]
                        if not last:
                            def smm(e, par=par, bi=bi, r0=r0, r1=r1, kt=kt):
                                ins = None
                                for h in range(4):
                                    ins = e.matmul(ps(6)[:, h * 128:(h + 1) * 128],
                                                   Ktok[kt][r0:r1, h * 128:(h + 1) * 128],
                                                   vtok[par][r0:r1, bi, h * 128:(h + 1) * 128],
                                                   start=True, stop=True)
                                return ins
                            add("pe", smm, reads=[("Ktok", kt), ("vtok", par, bi)], writes=[("ps", 6)])
                            sn = 1 - so
                            cnt["S"] = cnt.get("S", 0) + 1
                            gb = gdec[par][:, :, c:c + 1].to_broadcast([128, 4, 128])
                            if cg == 0:
                                add("dve", lambda e, sn=sn, gb=gb: e.tensor_tensor(
                                    Sst[sn], ps(6).rearrange("p (h v) -> p h v", h=4), gb, ALU.mult),
                                    reads=[("ps", 6)] + [("gdec", par, h) for h in range(4)], writes=[("Sst", sn)])
                            else:
                                add("dve", lambda e, so=so: e.tensor_tensor(
                                    Ub, ps(6).rearrange("p (h v) -> p h v", h=4), Sst[so], ALU.add),
                                    reads=[("ps", 6), ("Sst", so)], writes=["Ub"])
                                add("pool", lambda e, sn=sn, gb=gb: e.tensor_tensor(Sst[sn], Ub, gb, ALU.mult),
                                    reads=["Ub"] + [("gdec", par, h) for h in range(4)], writes=[("Sst", sn)])
                            add("act", lambda e, sn=sn: e.activation(Sbf[sn], Sst[sn], AF.Copy),
                                reads=[("Sst", sn)], writes=[("Sbf", sn)])
                        for f_ in flush_now:
                            f_()
                        add("act", lambda e, r0=r0, r1=r1, ob=ob: e.activation(
                            osq[r0:r1, :, :], ps_t[r0:r1, ob, :].rearrange("p (h v) -> p h v", h=4), AF.Square),
                            reads=[("ps", ob)], writes=[("osq", ce)])
                        add("dve", lambda e, r0=r0, r1=r1: e.reduce_sum(ssD[r0:r1, 0, :], osq[r0:r1, :, :], AX.X),
                            reads=[("osq", ce)], writes=[("ssD", 0, ce)])
                        add("act", lambda e, r0=r0, r1=r1: e.activation(ssD[r0:r1, 1, :], ssD[r0:r1, 0, :], AF.Ln,
                                                                        bias=EPS, scale=1.0 / 128),
                            reads=[("ssD", 0, ce)], writes=[("ssD", 1, ce)])
                        add("act", lambda e, r0=r0, r1=r1: e.activation(ssD[r0:r1, 2, :], ssD[r0:r1, 1, :], AF.Exp, scale=-0.5),
                            reads=[("ssD", 1, ce)], writes=[("ssD", 2, ce)])
                        add("dve", lambda e, r0=r0, r1=r1, ob=ob: e.tensor_tensor(
                            onb[r0:r1, :, :], ps_t[r0:r1, ob, :].rearrange("p (h v) -> p h v", h=4),
                            ssD[r0:r1, 2, :].unsqueeze(2).to_broadcast([64, 4, 128]), ALU.mult),
                            reads=[("ps", ob), ("ssD", 2, ce)], writes=[("onb", ce)])
                        def d_tail(r0=r0, r1=r1, ce=ce, par=par, tok0=tok0, tc0=tc0, tt=tt, c=c):
                            ts_ = rot("tp7", 2)

                            def tro(e, r0=r0, r1=r1, ts_=ts_):
                                ins = None
                                for h in range(4):
                                    ins = e.transpose(psb(7)[:, ts_ * 256 + h * 64:ts_ * 256 + (h + 1) * 64],
                                                      onb[r0:r1, h, :], ident[r0:r1, r0:r1])
                                return ins
                            add("pe", tro, reads=[("onb", ce), "ident"], writes=[("ps", 7)])
                            add("dve", lambda e, ts_=ts_, par=par, tok0=tok0, tc0=tc0: e.scalar_tensor_tensor(
                                ocatT[:, 4:8, tok0 + tc0:tok0 + tc0 + 64],
                                psb(7)[:, ts_ * 256:(ts_ + 1) * 256].rearrange("p (h t) -> p h t", h=4),
                                hgw[:, 0:1], Gt[par][:, :, tc0:tc0 + 64], ALU.mult, ALU.mult),
                                reads=[("ps", 7), "hgw"] + [("Gt", par, h) for h in range(4)],
                                writes=[("ocatT", 4, tt, c)])
                        dpend.append(d_tail)
            for f_ in dpend:
                f_()
            del dpend[:]
            if b == 0:
                tap("ocatT", ocatT, [("ocatT", 4, tt, c) for tt in range(8) for c in range(4)] +
                    [("ocatT", h, u) for h in range(4) for u in range(8)])

            mark("D%d" % b)
            S.barrier()
            for i in range(NT):
                xs = rot("xt", 3)
                add("sp", lambda e, xs=xs, i=i, b=b: e.dma_start(out=xt[xs], in_=x[b, i * 128:(i + 1) * 128, :]),
                    writes=[("xt", xs)], chan=("xt", xs))
                pp = (0, 3)[rot("pp", 2)]

                def oproj(e, i=i, pp=pp):
                    ins = None
                    for half in range(2):
                        for kc in range(8):
                            ins = e.matmul(ps(pp + half), ocatT[:, kc, i * 128:(i + 1) * 128],
                                           wout[:, kc, half * 512:(half + 1) * 512],
                                           start=(kc == 0), stop=(kc == 7))
                    return ins
                add("pe", oproj, reads=["wout"], writes=[("ps", pp), ("ps", pp + 1)])
                x1s = rot("x1t", 2)
                add("dve", lambda e, xs=xs, x1s=x1s, pp=pp: e.tensor_tensor(
                    x1t[x1s].rearrange("p (a n) -> p a n", a=2), xt[xs].rearrange("p (a n) -> p a n", a=2),
                    ps_t[:, pp:pp + 2, :], ALU.add),
                    reads=[("xt", xs), ("ps", pp), ("ps", pp + 1)], writes=[("x1t", x1s)])
                si = rot("st", 8)
                ss, ln_, rs = stat(si)
                gi = b * NT + i
                add("act", lambda e, x1s=x1s, ss=ss: e.activation(junk, x1t[x1s], AF.Square, accum_out=ss),
                    reads=[("x1t", x1s)], writes=["junk", ("ss", si)])
                add("act", lambda e, ss=ss, ln_=ln_: e.activation(ln_, ss, AF.Ln, bias=EPS, scale=1.0 / D),
                    reads=[("ss", si)], writes=[("ln", si)])
                add("act", lambda e, gi=gi, ln_=ln_: e.activation(rstd2[:, gi:gi + 1], ln_, AF.Exp, scale=-0.5),
                    reads=[("ln", si)], writes=[("rstd2", gi)])
                add("sp", lambda e, x1s=x1s, i=i, b=b: e.dma_start(out=out[b, i * 128:(i + 1) * 128, :], in_=x1t[x1s]),
                    reads=[("x1t", x1s)], chan=("x1st", x1s))
                if b == 0 and i == 0:
                    tap("x1", x1t[x1s], [("x1t", x1s)])
            S.barrier()
            mark("E%d" % b)

        mark("P1")
        HC = NFC // 2
        WB = [0, 3, 11, 16, NFC]

        def wpiece(jc):
            for p_ in range(4):
                if WB[p_] <= jc < WB[p_ + 1]:
                    return p_

        def ld_wup(p_):
            c0, c1 = WB[p_] * 128, WB[p_ + 1] * 128
            for part in range(2):
                add("pool", lambda e, c0=c0, c1=c1, part=part: e.dma_start(
                    out=wup[:, :, part * FF + c0:part * FF + c1],
                    in_=w_up[:, part * FF + c0:part * FF + c1].rearrange("(k p) n -> p k n", p=128)),
                    writes=[("wup", p_, part)], chan=("wup", p_, part))

        def ld_wdown(hf_):
            add("pool", lambda e, hf_=hf_: e.dma_start(
                out=wdown[:, hf_ * HC:(hf_ + 1) * HC, :],
                in_=w_down[hf_ * HC * 128:(hf_ + 1) * HC * 128, :].rearrange("(c p) n -> p c n", p=128)),
                writes=[("wdown", hf_)], chan=("wdown", hf_))

        ld_wup(0); ld_wup(1); ld_wdown(0); ld_wup(2); ld_wup(3); ld_wdown(1)
        mark("W2")
        for b in range(NB):
            for j in range(8):
                xs = rot("x1p", 2)
                gi0 = b * NT + 2 * j
                add("sp", lambda e, xs=xs, j=j, b=b: e.dma_start(
                    out=x1p[xs], in_=out[b, j * 256:(j + 1) * 256, :].rearrange("(s p) d -> p s d", p=128)),
                    writes=[("x1p", xs)], chan=("x1ld", xs))
                for s in range(2):
                    add("dve", lambda e, xs=xs, s=s, gi0=gi0: e.tensor_scalar(
                        h2n[xs][:, s, :], x1p[xs][:, s, :], rstd2[:, gi0 + s:gi0 + s + 1], None, ALU.mult),
                        reads=[("x1p", xs)], writes=[("h2n", xs, s)])
                hx = xs
                for kh in range(2):
                    tb = 2

                    def trh(e, xs=xs, kh=kh, tb=tb):
                        ins = None
                        for kq in range(4):
                            kc = kh * 4 + kq
                            for s in range(2):
                                ins = e.transpose(psb(tb)[:, kq * 256 + s * 128:kq * 256 + (s + 1) * 128],
                                                  h2n[xs][:, s, kc * 128:(kc + 1) * 128], ident[:, :])
                        return ins
                    add("pe", trh, reads=[("h2n", xs, 0), ("h2n", xs, 1)], writes=[("ps", tb)])
                    add("dve", lambda e, hx=hx, kh=kh, tb=tb: e.tensor_tensor(
                        h2T[hx][:, kh * 4:(kh + 1) * 4, :],
                        psb(tb)[:, 0:1024].rearrange("p (k t) -> p k t", k=4),
                        ln2c[:, kh * 4:(kh + 1) * 4].unsqueeze(2).to_broadcast([128, 4, 256]), ALU.mult),
                        reads=[("ps", tb)], writes=[("h2T", hx, kh)])
                gk_of = {}

                def emit_down(jc, gk_of=gk_of):
                    gk = gk_of[jc]

                    def dmm(e, jc=jc, gk=gk):
                        ins = None
                        for s in range(2):
                            for half in range(2):
                                ins = e.matmul(ps(4 + 2 * s + half), gbuf[gk][:, s * 128:(s + 1) * 128],
                                               wdown[:, jc, half * 512:(half + 1) * 512],
                                               start=(jc == 0), stop=(jc == NFC - 1))
                        return ins
                    add("pe", dmm, reads=[("g", gk), ("wdown", jc // HC)], writes=[("ps", 4), ("ps", 5), ("ps", 6), ("ps", 7)])

                for jc in range(NFC):
                    ub = (0, 1, 3)[rot("uv", 3)]

                    def upmm(e, jc=jc, ub=ub, hx=hx):
                        ins = None
                        for part in range(2):
                            for kc in range(8):
                                ins = e.matmul(ps(ub)[:, part * 256:(part + 1) * 256],
                                               wup[:, kc, part * FF + jc * 128:part * FF + (jc + 1) * 128],
                                               h2T[hx][:, kc, :], start=(kc == 0), stop=(kc == 7))
                        return ins
                    add("pe", upmm, reads=[("wup", wpiece(jc), 0), ("wup", wpiece(jc), 1), ("h2T", hx, 0), ("h2T", hx, 1)],
                        writes=[("ps", ub)])
                    ai = rot("acc2", 3)
                    a = accb[ai]
                    add("act", lambda e, a=a, ub=ub, jc=jc: e.activation(
                        a, ps(ub)[:, 0:256], AF.Identity, scale=cw[:, 2, jc:jc + 1], bias=cb[:, jc:jc + 1]),
                        reads=[("ps", ub)], writes=[("acc2", ai)])
                    add("dve", lambda e, a=a, ub=ub, jc=jc: e.scalar_tensor_tensor(
                        a[:, 1:256], ps(ub)[:, 0:255], cw[:, 1, jc:jc + 1], a[:, 1:256], ALU.mult, ALU.add),
                        reads=[("ps", ub), ("acc2", ai)], writes=[("acc2", ai)])
                    add("dve", lambda e, a=a, ub=ub, jc=jc: e.scalar_tensor_tensor(
                        a[:, 2:256], ps(ub)[:, 0:254], cw[:, 0, jc:jc + 1], a[:, 2:256], ALU.mult, ALU.add),
                        reads=[("ps", ub), ("acc2", ai)], writes=[("acc2", ai)])
                    if j > 0:
                        add("dve", lambda e, a=a, jc=jc: e.scalar_tensor_tensor(
                            a[:, 0:1], halo[:, jc, 1:2], cw[:, 1, jc:jc + 1], a[:, 0:1], ALU.mult, ALU.add),
                            reads=[("halo", jc), ("acc2", ai)], writes=[("acc2", ai)])
                        add("dve", lambda e, a=a, jc=jc: e.scalar_tensor_tensor(
                            a[:, 0:2], halo[:, jc, 0:2], cw[:, 0, jc:jc + 1], a[:, 0:2], ALU.mult, ALU.add),
                            reads=[("halo", jc), ("acc2", ai)], writes=[("acc2", ai)])
                    add("act", lambda e, ub=ub, jc=jc: e.activation(halo[:, jc, :], ps(ub)[:, 254:256], AF.Copy),
                        reads=[("ps", ub)], writes=[("halo", jc)])
                    sk = rot("sl", 2)
                    add("act", lambda e, a=a, sk=sk: e.activation(slb[sk], a, AF.Silu),
                        reads=[("acc2", ai)], writes=[("sl", sk)])
                    gk = rot("g", 4)
                    gk_of[jc] = gk
                    add("dve", lambda e, sk=sk, gk=gk, ub=ub: e.tensor_tensor(
                        gbuf[gk], slb[sk], ps(ub)[:, 256:512], ALU.mult),
                        reads=[("sl", sk), ("ps", ub)], writes=[("g", gk)])
                    if jc >= 2:
                        emit_down(jc - 2)
                emit_down(NFC - 2)
                emit_down(NFC - 1)
                for s in range(2):
                    add("dve", lambda e, xs=xs, s=s: e.tensor_tensor(
                        x1p[xs][:, s, :].rearrange("p (a n) -> p a n", a=2),
                        x1p[xs][:, s, :].rearrange("p (a n) -> p a n", a=2),
                        ps_t[:, 4 + 2 * s:6 + 2 * s, :], ALU.add),
                        reads=[("x1p", xs), ("ps", 4 + 2 * s), ("ps", 5 + 2 * s)], writes=[("x1p", xs)])
                add("sp", lambda e, xs=xs, j=j, b=b: e.dma_start(
                    out=out[b, j * 256:(j + 1) * 256, :].rearrange("(s p) d -> p s d", p=128), in_=x1p[xs]),
                    reads=[("x1p", xs)], chan=("ost", xs))
        mark("END")
        lim = None
        if limit_mark is not None:
            lim = marks[limit_mark]
        if marks_out is not None:
            marks_out.update(marks)
        S.emit(limit=lim)
    return nc


_PARAM_NAMES = ["ln1_w", "w_in", "q_norm_w", "k_norm_w", "lam_q1", "lam_k1", "lam_q2", "lam_k2",
                "diff_subln_w", "hgrn_lb_logits", "hgrn_norm_w", "w_out", "ln2_w", "w_up", "conv_w",
                "conv_b", "w_down"]


def _core_inputs(inputs, core):
    m = {"x": np.ascontiguousarray(inputs["x"][core * NB:(core + 1) * NB], dtype=np.float32)}
    for n in _PARAM_NAMES:
        a = np.asarray(inputs[n], dtype=np.float32)
        if n == "hgrn_lb_logits":
            m[n] = np.ascontiguousarray(a)
        elif n in ("w_in", "w_out", "w_up", "w_down", "conv_w"):
            m[n] = np.ascontiguousarray(a[0])
        else:
            m[n] = np.ascontiguousarray(a.reshape(1, -1))
    return m


def kernel(**inputs):
    nc = build_program()
    in_maps = [_core_inputs(inputs, c) for c in range(NCORES)]
    res = run_bass_kernel_spmd(nc, in_maps, core_ids=list(range(NCORES)))
    outs = [np.asarray(r["out"], dtype=np.float32) for r in res.results]
    return np.concatenate(outs, axis=0)
```

```python
import contextlib
import math

import numpy as np
import concourse.bass as bass
import concourse.mybir as mybir
from concourse.bass_utils import run_bass_kernel_spmd

F32 = mybir.dt.float32
BF16 = mybir.dt.bfloat16
ALU = mybir.AluOpType
AF = mybir.ActivationFunctionType
AX = mybir.AxisListType

NCORES = 8
NB = 2
SEQ = 2048
D = 1024
NT = SEQ // 128
FF = 2816
NFC = FF // 128
INC = 3584
EPS = 1e-6
LAM_INIT = 0.8 - 0.6 * math.exp(-0.3 * 0)
ENGS = ("pe", "act", "dve", "pool", "sp")


class _Op:
    __slots__ = ("eng", "fn", "deps", "signals", "sig", "chan", "chan_val")


class Sched:
    def __init__(self, nc):
        self.nc = nc
        self.ops = []
        self.eng_ops = {e: [] for e in ENGS}
        self.last_writer = {}
        self.readers = {}
        self.chan_count = {}
        self.chan_last = {}
        self.eng_last = {}
        self.bar = set()

    def add(self, eng, fn, reads=(), writes=(), chan=None):
        op = _Op()
        op.eng = eng; op.fn = fn; op.chan = chan
        op.signals = False; op.sig = None; op.chan_val = None
        writes = list(writes) + [k for k in reads if isinstance(k, tuple) and k[0] == "ps"]
        deps = set(self.bar)
        for k in reads:
            w = self.last_writer.get(k)
            if w is not None:
                deps.add(w)
        for k in writes:
            w = self.last_writer.get(k)
            if w is not None:
                deps.add(w)
            for r in self.readers.get(k, ()):
                deps.add(r)
        for k in reads:
            self.readers.setdefault(k, []).append(op)
        for k in writes:
            self.last_writer[k] = op
            self.readers[k] = []
        deps.discard(op)
        if eng == "pe":
            deps = {d for d in deps if not (d.eng == "pe" and d.chan is None)}
        op.deps = deps
        if chan is not None:
            self.chan_count[chan] = self.chan_count.get(chan, 0) + 16
            op.chan_val = self.chan_count[chan]
            self.chan_last[chan] = op
        else:
            self.eng_last[eng] = op
        self.ops.append(op)
        self.eng_ops[eng].append(op)
        return op

    def barrier(self):
        self.bar = set(self.eng_last.values()) | set(self.chan_last.values())
        self.last_writer = {}
        self.readers = {}

    def emit(self, final_wait_eng="sp", limit=None):
        nc = self.nc
        if limit is not None:
            kept = self.ops[:limit]
            self.ops = kept
            ks = set(kept)
            self.eng_ops = {e: [o for o in self.eng_ops[e] if o in ks] for e in ENGS}
            self.chan_count = {}
            for o in kept:
                if o.chan is not None:
                    self.chan_count[o.chan] = max(self.chan_count.get(o.chan, 0), o.chan_val)
        for op in self.ops:
            for d in op.deps:
                if d.chan is None:
                    d.signals = True
        cnt = {e: 0 for e in ENGS}
        for op in self.ops:
            if op.chan is None and op.signals:
                cnt[op.eng] += 1
                op.sig = cnt[op.eng]
        with contextlib.ExitStack() as st:
            esem = {e: st.enter_context(nc.semaphore("s_" + e)) for e in ENGS}
            csem = {}
            for i, c in enumerate(self.chan_count):
                csem[c] = st.enter_context(nc.semaphore("c%d" % i))
            block = st.enter_context(nc.Block())
            handles = {"pe": block.tensor, "act": block.scalar, "dve": block.vector,
                       "pool": block.gpsimd, "sp": block.sync}
            for e in ENGS:
                ops = self.eng_ops[e]
                if not ops and e != final_wait_eng:
                    continue

                def body(eng, ops=ops, e=e):
                    known = {}
                    for op in ops:
                        need = {}
                        for d in op.deps:
                            if d.chan is not None:
                                key = ("c", d.chan); val = d.chan_val
                            else:
                                key = ("e", d.eng); val = d.sig
                            if val > need.get(key, 0):
                                need[key] = val
                        for key, val in need.items():
                            if known.get(key, 0) >= val:
                                continue
                            known[key] = val
                            sem = csem[key[1]] if key[0] == "c" else esem[key[1]]
                            eng.wait_ge(sem, val)
                        ins = op.fn(eng)
                        if op.chan is not None:
                            ins.then_inc(csem[op.chan], 16)
                        elif op.signals:
                            ins.then_inc(esem[e], 1)
                    if e == final_wait_eng:
                        for c, v in self.chan_count.items():
                            if known.get(("c", c), 0) < v:
                                eng.wait_ge(csem[c], v)

                handles[e](body)


class Arena:
    def __init__(self, ap_f32, nwords):
        self.ap = ap_f32
        self.n = nwords
        self.off = 0

    def alloc(self, free_shape, dtype):
        n = 1
        for s in free_shape:
            n *= s
        nbytes = n * (2 if dtype == BF16 else 4)
        nw = (nbytes + 3) // 4
        nw = (nw + 7) // 8 * 8
        assert self.off + nw <= self.n, ("arena overflow", self.off, nw, self.n)
        v = self.ap[:, self.off:self.off + nw]
        self.off += nw
        if dtype == BF16:
            v = v.bitcast(BF16)
        v = v[:, 0:n]
        if len(free_shape) == 2:
            v = v.rearrange("p (a b) -> p a b", a=free_shape[0])
        elif len(free_shape) == 3:
            v = v.rearrange("p (a b c) -> p a b c", a=free_shape[0], b=free_shape[1])
        return v


def build_program(taps=None, limit_mark=None, marks_out=None):
    nc = bass.Bass("TRN2", target_bir_lowering=False)

    def din(name, shape):
        return nc.dram_tensor(name, shape, F32, kind="ExternalInput").ap()

    x = din("x", [NB, SEQ, D])
    ln1_w = din("ln1_w", [1, D])
    w_in = din("w_in", [D, INC])
    q_norm_w = din("q_norm_w", [1, 64])
    k_norm_w = din("k_norm_w", [1, 64])
    lam_q1 = din("lam_q1", [1, 64])
    lam_k1 = din("lam_k1", [1, 64])
    lam_q2 = din("lam_q2", [1, 64])
    lam_k2 = din("lam_k2", [1, 64])
    diff_subln_w = din("diff_subln_w", [1, 128])
    hgrn_lb_logits = din("hgrn_lb_logits", [2, 512])
    hgrn_norm_w = din("hgrn_norm_w", [1, 128])
    w_out = din("w_out", [D, D])
    ln2_w = din("ln2_w", [1, D])
    w_up = din("w_up", [D, 2 * FF])
    conv_w = din("conv_w", [3, FF])
    conv_b = din("conv_b", [1, FF])
    w_down = din("w_down", [FF, D])
    out = nc.dram_tensor("out", [NB, SEQ, D], F32, kind="ExternalOutput").ap()
    tap_t = {}
    if taps:
        for name, (shape, dt_) in taps.items():
            tap_t[name] = nc.dram_tensor("tap_" + name, shape, dt_, kind="ExternalOutput").ap()

    es = contextlib.ExitStack()
    with es:
        def sb(name, shape, dt_):
            return es.enter_context(nc.sbuf_tensor(name, shape, dt_))

        identf = sb("identf", [128, 128], F32)
        ident = sb("ident", [128, 128], BF16)
        blockones = sb("blockones", [128, 128], BF16)
        mask2 = sb("mask2", [128, 64], F32)
        onescol = sb("onescol", [128, 1], F32)
        NSTG = 116
        stage = sb("stage", [128, 128], F32)
        cst = sb("cst", [128, NSTG], F32)
        ln1c = cst[:, 0:8]
        ln2c = cst[:, 8:16]
        cw = cst[:, 16:82].rearrange("p (j c) -> p j c", j=3)
        cb = cst[:, 82:104]
        lbl = cst[:, 104:112].rearrange("p (r h) -> p r h", r=2)
        hgw = cst[:, 115:116]
        gqk = sb("gqk", [128, 2], F32)
        subw = sb("subw", [128, 1], F32)
        lamv = sb("lamv", [128, 4, 64], F32)
        lamp = sb("lamp", [128, 2, 64], F32)
        lams = sb("lams", [128, 2], F32)
        neglam = sb("neglam", [128, 1], F32)
        lbt = sb("lbt", [128, 4], F32)
        lb = sb("lb", [128, 4], F32)
        ln1mlb = sb("ln1mlb", [128, 4], F32)
        rstd2 = sb("rstd2", [128, NB * NT], F32)
        st4 = sb("st4", [128, 3, 8], F32)
        carry = sb("carry", [128, 4], F32)

        nwords = nc.sbuf_bytes_remaining // 4 - 64
        arena_t = sb("arena", [128, nwords], F32)
        ps_t = es.enter_context(nc.psum_tensor("ps", [128, 8, 512], F32))

        def ps(bank):
            return ps_t[:, bank, :]

        def psb(bank):
            return ps_t[:, bank, :].bitcast(BF16)

        S = Sched(nc)
        add = S.add
        cnt = {}
        marks = {}

        def mark(name):
            marks[name] = len(S.ops)

        def rot(name, n):
            v = cnt.get(name, 0)
            cnt[name] = v + 1
            return v % n

        def tap(name, src_ap, reads):
            if name in tap_t:
                add("sp", lambda e: e.dma_start(out=tap_t[name], in_=src_ap), reads=reads,
                    chan=("tap", name))

        def ld_small(dst, src, key):
            add("sp", lambda e: e.dma_start(out=dst, in_=src), writes=[key], chan=("c", key))

        add("pool", lambda e: e.memset(stage[:, :], 0.0), writes=["stage0"])
        srows = [
            (0, 8, ln1_w.rearrange("o (k p) -> (o k) p", p=128)),
            (8, 8, ln2_w.rearrange("o (k p) -> (o k) p", p=128)),
            (16, 66, conv_w.rearrange("j (c p) -> (j c) p", p=128)),
            (82, 22, conv_b.rearrange("o (c p) -> (o c) p", p=128)),
            (104, 8, hgrn_lb_logits.rearrange("r (h k) -> (r h) k", k=128)),
            (114, 1, diff_subln_w),
            (115, 1, hgrn_norm_w),
        ]
        skeys = ["stage0"]
        for r0_, n_, src_ in srows:
            add("sp", lambda e, r0_=r0_, n_=n_, src_=src_: e.dma_start(out=stage[r0_:r0_ + n_, :], in_=src_),
                reads=["stage0"], writes=[("stage", r0_)], chan=("c", "stage", r0_))
            skeys.append(("stage", r0_))
        for c in range(2):
            for r0_, src_ in ((112, q_norm_w), (113, k_norm_w)):
                add("sp", lambda e, r0_=r0_, src_=src_, c=c: e.dma_start(
                    out=stage[r0_:r0_ + 1, c * 64:(c + 1) * 64], in_=src_),
                    reads=["stage0"], writes=[("stage", r0_, c)], chan=("c", "stage", r0_, c))
                skeys.append(("stage", r0_, c))
        for i, lv in enumerate((lam_q1, lam_k1, lam_q2, lam_k2)):
            ld_small(lamv[:, i, :], lv.partition_broadcast(128), ("lamv", i))

        add("pool", lambda e: e.memset(identf[:, :], 1.0), writes=["identf"])
        add("pool", lambda e: e.affine_select(out=identf[:, :], in_=identf[:, :], pattern=[[-1, 128]],
                                              compare_op=ALU.is_equal, fill=0.0, base=0,
                                              channel_multiplier=1),
            reads=["identf"], writes=["identf"])
        add("dve", lambda e: e.tensor_copy(ident[:, :], identf[:, :]), reads=["identf"], writes=["ident"])
        add("pe", lambda e: e.transpose(ps_t[:, 0, 0:NSTG], stage[0:NSTG, :], identf[0:NSTG, 0:NSTG]),
            reads=skeys + ["identf"], writes=[("ps", 0)])
        add("dve", lambda e: e.tensor_copy(cst[:, :], ps_t[:, 0, 0:NSTG]), reads=[("ps", 0)],
            writes=["ln1c", "ln2c", "cw", "cb", "lbl", "cst"])
        add("pool", lambda e: e.memset(mask2[:, :], 1.0), writes=["mask2"])
        for hf_ in range(2):
            add("pool", lambda e, hf_=hf_: e.affine_select(
                out=mask2[hf_ * 64:(hf_ + 1) * 64, :], in_=mask2[hf_ * 64:(hf_ + 1) * 64, :],
                pattern=[[1, 64]], compare_op=ALU.is_ge, fill=0.0, base=0, channel_multiplier=-1),
                reads=["mask2"], writes=["mask2"])
        add("dve", lambda e: e.memset(blockones[:, :], 0.0), writes=["blockones"])
        for hf_ in range(2):
            add("dve", lambda e, hf_=hf_: e.memset(
                blockones[hf_ * 64:(hf_ + 1) * 64, hf_ * 64:(hf_ + 1) * 64], 1.0 / 64.0),
                writes=["blockones"])
        add("pool", lambda e: e.memset(onescol[:, :], 1.0), writes=["onescol"])
        add("dve", lambda e: e.tensor_scalar(gqk[:, 0:1], cst[:, 112:113], 0.125, None, ALU.mult),
            reads=["cst"], writes=["gqk"])
        add("dve", lambda e: e.tensor_copy(gqk[:, 1:2], cst[:, 113:114]), reads=["cst"], writes=["gqk"])
        add("dve", lambda e: e.tensor_scalar(subw[:, :], cst[:, 114:115], 1.0 - LAM_INIT, None, ALU.mult),
            reads=["cst"], writes=["subw"])
        add("dve", lambda e: e.tensor_tensor(lamp[:, 0, :], lamv[:, 0, :], lamv[:, 1, :], ALU.mult),
            reads=[("lamv", 0), ("lamv", 1)], writes=[("lamp", 0)])
        add("dve", lambda e: e.tensor_tensor(lamp[:, 1, :], lamv[:, 2, :], lamv[:, 3, :], ALU.mult),
            reads=[("lamv", 2), ("lamv", 3)], writes=[("lamp", 1)])
        add("dve", lambda e: e.reduce_sum(lams[:, :], lamp[:, :, :], AX.X),
            reads=[("lamp", 0), ("lamp", 1)], writes=["lams"])
        add("act", lambda e: e.activation(lams[:, :], lams[:, :], AF.Exp), reads=["lams"], writes=["lams"])
        add("dve", lambda e: e.tensor_tensor(neglam[:, :], lams[:, 1:2], lams[:, 0:1], ALU.subtract),
            reads=["lams"], writes=["neglam"])
        add("dve", lambda e: e.tensor_scalar(neglam[:, :], neglam[:, :], -LAM_INIT, None, ALU.add),
            reads=["neglam"], writes=["neglam"])
        add("dve", lambda e: e.tensor_tensor(lbt[:, :], lbl[:, 1, :], lbl[:, 0, :], ALU.subtract),
            reads=["lbl"], writes=["lbt"])
        add("act", lambda e: e.activation(lbt[:, :], lbt[:, :], AF.Exp), reads=["lbt"], writes=["lbt"])
        add("dve", lambda e: e.tensor_scalar(lbt[:, :], lbt[:, :], 1.0, None, ALU.add),
            reads=["lbt"], writes=["lbt"])
        add("dve", lambda e: e.reciprocal(lb[:, :], lbt[:, :]), reads=["lbt"], writes=["lb"])
        add("act", lambda e: e.activation(ln1mlb[:, :], lb[:, :], AF.Ln, bias=1.0, scale=-1.0),
            reads=["lb"], writes=["ln1mlb"])

        mark("consts")
        A1 = Arena(arena_t[:, :], nwords)
        wout = A1.alloc([8, D], BF16)
        winb = [A1.alloc([8, 512], BF16) for _ in range(4)]
        hT = A1.alloc([8, SEQ], BF16)
        ocatT = A1.alloc([8, SEQ], BF16)
        xt = [A1.alloc([D], F32) for _ in range(3)]
        junk = A1.alloc([D], BF16)
        hn = [A1.alloc([D], BF16) for _ in range(2)]
        x1t = [A1.alloc([D], F32) for _ in range(2)]
        local_base = A1.off
        qkT = [A1.alloc([2, SEQ], BF16) for _ in range(2)]
        vaug = A1.alloc([NT, 2, 130], BF16)
        pt = [A1.alloc([2, 256], BF16) for _ in range(3)]
        pd = [A1.alloc([2, 256], BF16) for _ in range(2)]
        sq = [A1.alloc([512], BF16) for _ in range(2)]
        lnms = [A1.alloc([512], F32) for _ in range(2)]
        t0 = [A1.alloc([2, 128], F32) for _ in range(2)]
        t1a = [A1.alloc([2, 128], F32) for _ in range(2)]
        od = [A1.alloc([2, 128], F32) for _ in range(2)]
        odn = [A1.alloc([2, 128], BF16) for _ in range(2)]
        junk2 = A1.alloc([128], BF16)
        rz = [A1.alloc([4], F32) for _ in range(2)]
        rzl = [A1.alloc([2], F32) for _ in range(2)]
        attn_end = A1.off
        A1.off = local_base
        he = [A1.alloc([256], F32) for _ in range(2)]
        hL1 = [A1.alloc([256], F32) for _ in range(2)]
        hL2 = [A1.alloc([256], F32) for _ in range(2)]
        ht1 = [A1.alloc([256], F32) for _ in range(2)]
        heg = [A1.alloc([256], F32) for _ in range(2)]
        hBx = [A1.alloc([264], F32) for _ in range(2)]
        QtT = [A1.alloc([4, 256], BF16) for _ in range(2)]
        KtT = [A1.alloc([4, 256], BF16) for _ in range(2)]
        Gt = [A1.alloc([4, 256], BF16) for _ in range(2)]
        vtok = [A1.alloc([2, 512], BF16) for _ in range(2)]
        gdec = [A1.alloc([4, 4], F32) for _ in range(2)]
        Ktok = [A1.alloc([512], BF16) for _ in range(2)]
        AT = [A1.alloc([4, 64], BF16) for _ in range(2)]
        Ub = A1.alloc([4, 128], F32)
        Sst = [A1.alloc([4, 128], F32) for _ in range(2)]
        Sbf = [A1.alloc([4, 128], BF16) for _ in range(2)]
        osq = A1.alloc([4, 128], F32)
        onb = A1.alloc([4, 128], BF16)
        ssD = A1.alloc([3, 4], F32)
        A1.off = max(A1.off, attn_end)

        A2 = Arena(arena_t[:, :], nwords)
        wup = A2.alloc([8, 2 * FF], BF16)
        wdown = A2.alloc([NFC, D], BF16)
        x1p = [A2.alloc([2, D], F32) for _ in range(2)]
        h2n = [A2.alloc([2, D], BF16) for _ in range(2)]
        h2T = [A2.alloc([8, 256], BF16) for _ in range(2)]
        accb = [A2.alloc([256], F32) for _ in range(3)]
        slb = [A2.alloc([256], F32) for _ in range(2)]
        gbuf = [A2.alloc([256], BF16) for _ in range(4)]
        halo = A2.alloc([NFC, 2], F32)

        add("pool", lambda e: e.dma_start(out=wout, in_=w_out.rearrange("(k p) n -> p k n", p=128)),
            writes=["wout"], chan="wout")

        def load_win(g, slot):
            add("pool", lambda e: e.dma_start(
                out=winb[slot], in_=w_in[:, g * 512:(g + 1) * 512].rearrange("(k p) n -> p k n", p=128)),
                writes=[("win", slot)], chan=("win", slot))

        def stat(i):
            return st4[:, 0, i:i + 1], st4[:, 1, i:i + 1], st4[:, 2, i:i + 1]

        for b in range(NB):
            for g in range(4):
                load_win(g, g)
            for i in range(NT):
                xs = rot("xt", 3)
                add("sp", lambda e, xs=xs, i=i, b=b: e.dma_start(out=xt[xs], in_=x[b, i * 128:(i + 1) * 128, :]),
                    writes=[("xt", xs)], chan=("xt", xs))
                si = rot("st", 8)
                ss, ln_, rs = stat(si)
                add("act", lambda e, xs=xs, ss=ss: e.activation(junk, xt[xs], AF.Square, accum_out=ss),
                    reads=[("xt", xs)], writes=["junk", ("ss", si)])
                add("act", lambda e, ss=ss, ln_=ln_: e.activation(ln_, ss, AF.Ln, bias=EPS, scale=1.0 / D),
                    reads=[("ss", si)], writes=[("ln", si)])
                add("act", lambda e, rs=rs, ln_=ln_: e.activation(rs, ln_, AF.Exp, scale=-0.5),
                    reads=[("ln", si)], writes=[("rs", si)])
                hs = rot("hn", 2)
                add("dve", lambda e, hs=hs, xs=xs, rs=rs: e.tensor_scalar(hn[hs], xt[xs], rs, None, ALU.mult),
                    reads=[("xt", xs), ("rs", si)], writes=[("hn", hs)])
                tb = (2, 7)[rot("tpA", 2)]

                def tr8(e, hs=hs, tb=tb):
                    ins = None
                    for kc in range(8):
                        ins = e.transpose(psb(tb)[:, kc * 128:(kc + 1) * 128],
                                          hn[hs][:, kc * 128:(kc + 1) * 128], ident[:, :])
                    return ins
                add("pe", tr8, reads=[("hn", hs), "ident"], writes=[("ps", tb)])
                add("dve", lambda e, tb=tb, i=i: e.tensor_tensor(
                    hT[:, :, i * 128:(i + 1) * 128],
                    psb(tb)[:, 0:1024].rearrange("p (k t) -> p k t", k=8),
                    ln1c[:, :].unsqueeze(2).to_broadcast([128, 8, 128]), ALU.mult),
                    reads=[("ps", tb), "ln1c"], writes=[("hT", i)])
            mark("A%d" % b)
            if b == 0:
                tap("hT", hT, [("hT", i) for i in range(NT)])

            for hp in range(2):
                S.barrier()
                if hp == 0:
                    add("dve", lambda e: e.memset(vaug[:, :, :, 128:129], 1.0), writes=["vaug_ones"])
                    add("dve", lambda e: e.memset(vaug[:, :, :, 129:130], 0.0), writes=["vaug_ones"])
                    for k_ in range(2):
                        add("dve", lambda e, k_=k_: e.memset(pd[k_][64:128, :, 0:64], 0.0),
                            writes=[("pdz", k_)])
                items = [(T, hh, which) for T in range(4) for hh in range(2) for which in range(2)]
                st_ = {}

                def b_front(k):
                    T, hh, which = items[k]
                    h = 2 * hp + hh
                    pb = (0, 1, 5, 6)[rot("pa", 4)]
                    j = rot("sq", 2)
                    st_[k] = (pb, j)

                    def proj(e, which=which, h=h, T=T, pb=pb):
                        ins = None
                        for kc in range(8):
                            ins = e.matmul(ps(pb), winb[which][:, kc, h * 128:(h + 1) * 128],
                                           hT[:, kc, T * 512:(T + 1) * 512],
                                           start=(kc == 0), stop=(kc == 7))
                        return ins
                    add("pe", proj, reads=[("win", which)] + [("hT", 4 * T + jj) for jj in range(4)],
                        writes=[("ps", pb)])
                    add("act", lambda e, j=j, pb=pb: e.activation(sq[j], ps(pb), AF.Square),
                        reads=[("ps", pb)], writes=[("sq", j)])

                def b_back(k):
                    T, hh, which = items[k]
                    pb, j = st_[k]
                    mb = (3, 4)[rot("ms", 2)]
                    add("pe", lambda e, j=j, mb=mb: e.matmul(ps(mb), blockones[:, :], sq[j],
                                                             start=True, stop=True),
                        reads=[("sq", j), "blockones"], writes=[("ps", mb)])
                    add("act", lambda e, j=j, mb=mb: e.activation(lnms[j], ps(mb), AF.Ln, bias=EPS),
                        reads=[("ps", mb)], writes=[("lnms", j)])
                    add("act", lambda e, j=j: e.activation(lnms[j], lnms[j], AF.Exp, scale=-0.5),
                        reads=[("lnms", j)], writes=[("lnms", j)])
                    add("dve", lambda e, j=j, pb=pb, which=which, hh=hh, T=T: e.scalar_tensor_tensor(
                        qkT[which][:, hh, T * 512:(T + 1) * 512], ps(pb), gqk[:, which:which + 1],
                        lnms[j], ALU.mult, ALU.mult),
                        reads=[("ps", pb), ("lnms", j), "gqk"], writes=[("qkT", which, hh, T)])

                for k in range(len(items)):
                    b_front(k)
                    if k >= 1:
                        b_back(k - 1)
                b_back(len(items) - 1)
                for i in range(NT):
                    pb = (0, 1, 5, 6)[rot("pa", 4)]

                    def projv(e, i=i, pb=pb, hp=hp):
                        ins = None
                        for kc in range(8):
                            ins = e.matmul(ps(pb)[:, 0:256], hT[:, kc, i * 128:(i + 1) * 128],
                                           winb[2][:, kc, hp * 256:(hp + 1) * 256],
                                           start=(kc == 0), stop=(kc == 7))
                        return ins
                    add("pe", projv, reads=[("win", 2), ("hT", i)], writes=[("ps", pb)])
                    add("act", lambda e, i=i, pb=pb: e.activation(
                        vaug[:, i, :, 0:128], ps(pb)[:, 0:256].rearrange("p (h e) -> p h e", h=2), AF.Copy),
                        reads=[("ps", pb)], writes=[("vaug", i)])
                mark("B%d_%d" % (b, hp))
                if b == 0 and hp == 0:
                    tap("qT", qkT[0], [("qkT", 0, hh, T) for hh in range(2) for T in range(4)])
                    tap("kT", qkT[1], [("qkT", 1, hh, T) for hh in range(2) for T in range(4)])
                    tap("vaug", vaug, [("vaug", i) for i in range(NT)] + ["vaug_ones"])
                if hp == 1:
                    for g, slot in ((4, 0), (5, 1), (6, 2)):
                        load_win(g, slot)

                pending = []
                for hh in range(2):
                    h = 2 * hp + hh
                    for u in range(8):
                        ab0 = 5
                        ns = 2 * u + 2
                        info = {}

                        def emit_qk(i, hh=hh, u=u, info=info):
                            scb = (0, 3)[rot("sc", 2)]
                            lo = 128 if i == 2 * u + 1 else 0
                            n = 256 - lo
                            info[i] = (scb, lo, n)

                            def qk(e, hh=hh, u=u, i=i, scb=scb, lo=lo, n=n):
                                ins = None
                                for c in range(2):
                                    ins = e.matmul(ps(scb + c)[:, 0:n],
                                                   qkT[1][c * 64:(c + 1) * 64, hh, i * 128:(i + 1) * 128],
                                                   qkT[0][c * 64:(c + 1) * 64, hh, u * 256 + lo:(u + 1) * 256],
                                                   start=True, stop=True)
                                return ins
                            add("pe", qk, reads=[("qkT", 1, hh, i // 4), ("qkT", 0, hh, u // 2)],
                                writes=[("ps", scb), ("ps", scb + 1)])

                        def emit_exp_pv(i, hh=hh, u=u, info=info, ab0=ab0):
                            scb, lo, n = info[i]
                            scv = ps_t[:, scb:scb + 2, 0:256]
                            if i < 2 * u:
                                pi = rot("pt", 3)
                                pbuf = pt[pi]; pkey = ("pt", pi)
                                add("act", lambda e, pbuf=pbuf, scv=scv: e.activation(pbuf, scv, AF.Exp),
                                    reads=[("ps", scb), ("ps", scb + 1)], writes=[pkey])
                                extra = []
                            else:
                                pi = rot("pd", 2)
                                pbuf = pd[pi]; pkey = ("pd", pi)
                                add("act", lambda e, pbuf=pbuf, scv=scv, n=n: e.activation(
                                    pbuf[0:64, :, 0:n], scv[0:64, :, 0:n], AF.Exp),
                                    reads=[("ps", scb), ("ps", scb + 1)], writes=[pkey])
                                add("act", lambda e, pbuf=pbuf, scv=scv, n=n: e.activation(
                                    pbuf[64:128, :, 64:n], scv[64:128, :, 64:n], AF.Exp),
                                    reads=[("ps", scb), ("ps", scb + 1)], writes=[pkey])
                                extra = [("pdz", pi)]
                            return pbuf, pkey, extra

                        def emit_pv(i, pbuf, pkey, extra, hh=hh, u=u, ab0=ab0):
                            jbs = (1,) if i == 2 * u + 1 else (0, 1)

                            def pv(e, pbuf=pbuf, i=i, u=u, hh=hh, jbs=jbs, ab0=ab0):
                                ins = None
                                for jb in jbs:
                                    off = 0 if i == 2 * u + 1 else jb * 128
                                    for c in range(2):
                                        ins = e.matmul(ps(ab0 + jb)[:, c * 256:c * 256 + 130],
                                                       pbuf[:, c, off:off + 128], vaug[:, i, hh, :],
                                                       start=(i == 0 and c == 0), stop=(i == 2 * u + jb),
                                                       skip_group_check=True)
                                return ins
                            add("pe", pv, reads=[pkey, ("vaug", i), "vaug_ones"] + extra,
                                writes=[("ps", ab0 + jb) for jb in jbs])

                        emit_qk(0)
                        for i in range(ns):
                            pbuf, pkey, extra = emit_exp_pv(i)
                            if i + 1 < ns:
                                emit_qk(i + 1)
                            emit_pv(i, pbuf, pkey, extra)
                            if i == 0 and pending:
                                pending.pop(0)()
                        k_ = rot("fin", 2)
                        accv = ps_t[:, ab0:ab0 + 2, :].rearrange("p j (c w) -> p j c w", c=2)
                        rzv = rz[k_].rearrange("p (j c) -> p j c", j=2)
                        add("dve", lambda e, accv=accv, rzv=rzv: e.reciprocal(rzv, accv[:, :, :, 128]),
                            reads=[("ps", ab0), ("ps", ab0 + 1)], writes=[("rz", k_)])
                        add("dve", lambda e, rzv=rzv, k_=k_: e.tensor_scalar(
                            rzl[k_], rzv[:, :, 1], neglam[:, 0:1], None, ALU.mult),
                            reads=[("rz", k_), "neglam"], writes=[("rzl", k_)])
                        add("dve", lambda e, accv=accv, rzv=rzv, k_=k_: e.tensor_tensor(
                            t0[k_], accv[:, :, 0, 0:128], rzv[:, :, 0:1].to_broadcast([128, 2, 128]), ALU.mult),
                            reads=[("ps", ab0), ("ps", ab0 + 1), ("rz", k_)], writes=[("t0", k_)])
                        add("dve", lambda e, accv=accv, k_=k_: e.tensor_tensor(
                            t1a[k_], accv[:, :, 1, 0:128],
                            rzl[k_].unsqueeze(2).to_broadcast([128, 2, 128]), ALU.mult),
                            reads=[("ps", ab0), ("ps", ab0 + 1), ("rzl", k_)], writes=[("t1a", k_)])
                        add("pool", lambda e, k_=k_: e.tensor_tensor(od[k_], t0[k_], t1a[k_], ALU.add),
                            reads=[("t0", k_), ("t1a", k_)], writes=[("od", k_)])
                        si = rot("st", 8)
                        si2 = rot("st", 8)
                        assert si2 == si + 1
                        ssw = st4[:, 0, si:si + 2]; lnw = st4[:, 1, si:si + 2]; rsw = st4[:, 2, si:si + 2]
                        for jb in range(2):
                            add("act", lambda e, k_=k_, jb=jb, si=si: e.activation(
                                junk2, od[k_][:, jb, :], AF.Square, accum_out=st4[:, 0, si + jb:si + jb + 1]),
                                reads=[("od", k_)], writes=["junk2", ("ss", si + jb)])
                        add("act", lambda e, ssw=ssw, lnw=lnw: e.activation(lnw, ssw, AF.Ln, bias=EPS, scale=1.0 / 128),
                            reads=[("ss", si), ("ss", si + 1)], writes=[("ln", si), ("ln", si + 1)])
                        add("act", lambda e, rsw=rsw, lnw=lnw: e.activation(rsw, lnw, AF.Exp, scale=-0.5),
                            reads=[("ln", si), ("ln", si + 1)], writes=[("rs", si), ("rs", si + 1)])
                        add("dve", lambda e, k_=k_, rsw=rsw: e.tensor_tensor(
                            odn[k_], od[k_], rsw.unsqueeze(2).to_broadcast([128, 2, 128]), ALU.mult),
                            reads=[("od", k_), ("rs", si), ("rs", si + 1)], writes=[("odn", k_)])

                        def fin_tail(k_=k_, h=h, u=u, b=b, hp=hp, hh=hh):
                            tb = (2, 7)[rot("tpC", 2)]

                            def tr2(e, k_=k_, tb=tb):
                                ins = None
                                for jb in range(2):
                                    ins = e.transpose(psb(tb)[:, jb * 128:(jb + 1) * 128], odn[k_][:, jb, :], ident[:, :])
                                return ins
                            add("pe", tr2, reads=[("odn", k_), "ident"], writes=[("ps", tb)])
                            add("act", lambda e, tb=tb, h=h, u=u: e.activation(
                                ocatT[:, h, u * 256:(u + 1) * 256], psb(tb)[:, 0:256], AF.Identity, scale=subw[:, 0:1]),
                                reads=[("ps", tb), "subw"], writes=[("ocatT", h, u)])
                        pending.append(fin_tail)
                        mark("Cu_%d_%d_%d_%d" % (b, hp, hh, u))
                while pending:
                    pending.pop(0)()

            mark("C%d" % b)
            S.barrier()
            SL_HQ, SL_HF, SL_HI, SL_HG = 3, 0, 1, 2
            dpend = []
            for tt in range(8):
                par = tt % 2
                tok0 = tt * 256
                hkeys = [("hT", 2 * tt), ("hT", 2 * tt + 1)]
                for bi in range(2):
                    i = 2 * tt + bi
                    pb = (0, 1, 3)[rot("pd3", 3)]

                    def projhi(e, i=i, pb=pb):
                        ins = None
                        for kc in range(8):
                            ins = e.matmul(ps(pb), hT[:, kc, i * 128:(i + 1) * 128], winb[SL_HI][:, kc, :],
                                           start=(kc == 0), stop=(kc == 7))
                        return ins
                    add("pe", projhi, reads=[("win", SL_HI), ("hT", i)], writes=[("ps", pb)])
                    add("act", lambda e, par=par, bi=bi, pb=pb: e.activation(vtok[par][:, bi, :], ps(pb), AF.Copy),
                        reads=[("ps", pb)], writes=[("vtok", par, bi)])
                for h in range(4):
                    s_ = h % 2

                    def projf(slot, pb, h=h, tok0=tok0):
                        def f(e):
                            ins = None
                            for kc in range(8):
                                ins = e.matmul(ps(pb)[:, 0:256], winb[slot][:, kc, h * 128:(h + 1) * 128],
                                               hT[:, kc, tok0:tok0 + 256], start=(kc == 0), stop=(kc == 7))
                            return ins
                        return f
                    pb = (0, 1, 3)[rot("pd3", 3)]
                    add("pe", projf(SL_HF, pb), reads=[("win", SL_HF)] + hkeys, writes=[("ps", pb)])
                    add("act", lambda e, s_=s_, pb=pb: e.activation(he[s_], ps(pb)[:, 0:256], AF.Exp, scale=-1.0),
                        reads=[("ps", pb)], writes=[("he", s_)])
                    add("act", lambda e, s_=s_: e.activation(hL1[s_], he[s_], AF.Ln, bias=1.0),
                        reads=[("he", s_)], writes=[("hL1", s_)])
                    add("act", lambda e, s_=s_, h=h: e.activation(hL2[s_], he[s_], AF.Ln, bias=1.0, scale=lb[:, h:h + 1]),
                        reads=[("he", s_), "lb"], writes=[("hL2", s_)])
                    add("dve", lambda e, s_=s_, pb=pb: e.tensor_tensor(ht1[s_], ps(pb)[:, 0:256], hL1[s_], ALU.add),
                        reads=[("ps", pb), ("hL1", s_)], writes=[("ht1", s_)])
                    add("pool", lambda e, s_=s_: e.tensor_tensor(hL2[s_], hL2[s_], hL1[s_], ALU.subtract),
                        reads=[("hL2", s_), ("hL1", s_)], writes=[("hL2", s_)])
                    if tt == 0:
                        add("pool", lambda e, s_=s_: e.memset(hBx[s_][:, 0:1], 0.0), writes=[("hBx", s_)])
                    else:
                        add("pool", lambda e, s_=s_, h=h: e.tensor_copy(hBx[s_][:, 0:1], carry[:, h:h + 1]),
                            reads=[("carry", h)], writes=[("hBx", s_)])
                    add("dve", lambda e, s_=s_: e.tensor_tensor_scan(
                        hBx[s_][:, 1:257], onescol[:, 0:1].to_broadcast([128, 256]), hL2[s_],
                        hBx[s_][:, 0:1], ALU.mult, ALU.add),
                        reads=[("hL2", s_), ("hBx", s_), "onescol"], writes=[("hBx", s_)])
                    add("pool", lambda e, s_=s_, h=h: e.tensor_copy(carry[:, h:h + 1], hBx[s_][:, 256:257]),
                        reads=[("hBx", s_)], writes=[("carry", h)])
                    add("act", lambda e, s_=s_, h=h: e.activation(ht1[s_], ht1[s_], AF.Exp, scale=-1.0,
                                                                  bias=ln1mlb[:, h:h + 1]),
                        reads=[("ht1", s_), "ln1mlb"], writes=[("ht1", s_)])
                    add("dve", lambda e, s_=s_: e.tensor_tensor(
                        he[s_].rearrange("p (c t) -> p c t", c=4),
                        hBx[s_][:, 1:257].rearrange("p (c t) -> p c t", c=4),
                        hBx[s_][:, 0:256:64].unsqueeze(2).to_broadcast([128, 4, 64]), ALU.subtract),
                        reads=[("hBx", s_), ("he", s_), ("hL2", s_)], writes=[("he", s_)])
                    add("act", lambda e, s_=s_: e.activation(hL1[s_], he[s_], AF.Exp),
                        reads=[("he", s_), ("ht1", s_)], writes=[("hL1", s_)])
                    add("act", lambda e, s_=s_: e.activation(he[s_], he[s_], AF.Exp, scale=-1.0),
                        reads=[("he", s_), ("hL1", s_)], writes=[("he", s_)])
                    pb2 = (0, 1, 3)[rot("pd3", 3)]
                    add("pe", projf(SL_HQ, pb2), reads=[("win", SL_HQ)] + hkeys, writes=[("ps", pb2)])
                    add("dve", lambda e, s_=s_, pb2=pb2, par=par, h=h: e.tensor_tensor(
                        QtT[par][:, h, :], ps(pb2)[:, 0:256], hL1[s_], ALU.mult),
                        reads=[("ps", pb2), ("hL1", s_)], writes=[("QtT", par, h)])
                    add("pool", lambda e, s_=s_, par=par, h=h: e.tensor_tensor(
                        KtT[par][:, h, :], ht1[s_], he[s_], ALU.mult),
                        reads=[("ht1", s_), ("he", s_)], writes=[("KtT", par, h)])
                    add("pool", lambda e, s_=s_, par=par, h=h: e.tensor_copy(
                        gdec[par][:, h, :], hL1[s_].rearrange("p (c t) -> p c t", c=4)[:, :, 63]),
                        reads=[("hL1", s_)], writes=[("gdec", par, h)])
                    pb3 = (0, 1, 3)[rot("pd3", 3)]
                    add("pe", projf(SL_HG, pb3), reads=[("win", SL_HG)] + hkeys, writes=[("ps", pb3)])
                    add("act", lambda e, s_=s_, pb3=pb3: e.activation(heg[s_], ps(pb3)[:, 0:256], AF.Exp, scale=-1.0),
                        reads=[("ps", pb3)], writes=[("heg", s_)])
                    add("act", lambda e, s_=s_: e.activation(heg[s_], heg[s_], AF.Ln, bias=1.0),
                        reads=[("heg", s_)], writes=[("heg", s_)])
                    add("act", lambda e, s_=s_: e.activation(heg[s_], heg[s_], AF.Exp, scale=-1.0),
                        reads=[("heg", s_)], writes=[("heg", s_)])
                    add("dve", lambda e, s_=s_, pb3=pb3, par=par, h=h: e.tensor_tensor(
                        Gt[par][:, h, :], ps(pb3)[:, 0:256], heg[s_], ALU.mult),
                        reads=[("ps", pb3), ("heg", s_)], writes=[("Gt", par, h)])
                if b == 0 and tt == 0:
                    tap("QtT", QtT[0], [("QtT", 0, h) for h in range(4)])
                    tap("KtT", KtT[0], [("KtT", 0, h) for h in range(4)])
                    tap("Gt", Gt[0], [("Gt", 0, h) for h in range(4)])
                for bi in range(2):
                    kt = rot("ktok", 2)

                    def trk(e, par=par, bi=bi):
                        ins = None
                        for h in range(4):
                            ins = e.transpose(psb(7)[:, 512 + h * 128:512 + (h + 1) * 128],
                                              KtT[par][:, h, bi * 128:(bi + 1) * 128], ident[:, :])
                        return ins
                    add("pe", trk, reads=[("KtT", par, h) for h in range(4)] + ["ident"], writes=[("ps", 7)])
                    add("act", lambda e, kt=kt: e.activation(Ktok[kt], psb(7)[:, 512:1024], AF.Copy),
                        reads=[("ps", 7)], writes=[("Ktok", kt)])
                    for ce in range(2):
                        c = 2 * bi + ce
                        cg = 4 * tt + c
                        r0, r1 = ce * 64, ce * 64 + 64
                        tc0 = c * 64
                        ah = rot("A", 2)
                        at = rot("AT", 2)

                        def amm(e, par=par, tc0=tc0, r0=r0, r1=r1, ah=ah):
                            ins = None
                            for h in range(4):
                                ins = e.matmul(ps_t[r0:r1, 4, ah * 256 + h * 64:ah * 256 + (h + 1) * 64],
                                               KtT[par][:, h, tc0:tc0 + 64], QtT[par][:, h, tc0:tc0 + 64],
                                               start=True, stop=True)
                            return ins
                        add("pe", amm, reads=[("KtT", par, h) for h in range(4)] + [("QtT", par, h) for h in range(4)],
                            writes=[("ps", 4)])
                        add("dve", lambda e, r0=r0, r1=r1, ah=ah, at=at: e.tensor_tensor(
                            AT[at][r0:r1, :, :],
                            ps_t[r0:r1, 4, ah * 256:(ah + 1) * 256].rearrange("p (h t) -> p h t", h=4),
                            mask2[r0:r1, :].unsqueeze(1).to_broadcast([64, 4, 64]), ALU.mult),
                            reads=[("ps", 4), "mask2"], writes=[("AT", at, ce)])
                        so = cnt.get("S", 0) % 2
                        ob = (5, 2)[cg % 2]

                        def omm(e, par=par, bi=bi, tc0=tc0, r0=r0, r1=r1, at=at, cg=cg, so=so, ob=ob):
                            ins = None
                            for h in range(4):
                                ins = e.matmul(ps_t[r0:r1, ob, h * 128:(h + 1) * 128], AT[at][r0:r1, h, :],
                                               vtok[par][r0:r1, bi, h * 128:(h + 1) * 128],
                                               start=True, stop=(cg == 0))
                                if cg > 0:
                                    ins = e.matmul(ps_t[r0:r1, ob, h * 128:(h + 1) * 128],
                                                   QtT[par][:, h, tc0:tc0 + 64], Sbf[so][:, h, :],
                                                   start=False, stop=True)
                            return ins
                        add("pe", omm, reads=[("AT", at, ce), ("vtok", par, bi), ("Sbf", so)] +
                            [("QtT", par, h) for h in range(4)], writes=[("ps", ob)])
                        last = (cg == 31)
                        flush_now = list(dpend)
                        del dpend[:]
                        if not last:
                            def smm(e, par=par, bi=bi, r0=r0, r1=r1, kt=kt):
                                ins = None
                                for h in range(4):
                                    ins = e.matmul(ps(6)[:, h * 128:(h + 1) * 128],
                                                   Ktok[kt][r0:r1, h * 128:(h + 1) * 128],
                                                   vtok[par][r0:r1, bi, h * 128:(h + 1) * 128],
                                                   start=True, stop=True)
                                return ins
                            add("pe", smm, reads=[("Ktok", kt), ("vtok", par, bi)], writes=[("ps", 6)])
                            sn = 1 - so
                            cnt["S"] = cnt.get("S", 0) + 1
                            gb = gdec[par][:, :, c:c + 1].to_broadcast([128, 4, 128])
                            if cg == 0:
                                add("dve", lambda e, sn=sn, gb=gb: e.tensor_tensor(
                                    Sst[sn], ps(6).rearrange("p (h v) -> p h v", h=4), gb, ALU.mult),
                                    reads=[("ps", 6)] + [("gdec", par, h) for h in range(4)], writes=[("Sst", sn)])
                            else:
                                add("dve", lambda e, so=so: e.tensor_tensor(
                                    Ub, ps(6).rearrange("p (h v) -> p h v", h=4), Sst[so], ALU.add),
                                    reads=[("ps", 6), ("Sst", so)], writes=["Ub"])
                                add("pool", lambda e, sn=sn, gb=gb: e.tensor_tensor(Sst[sn], Ub, gb, ALU.mult),
                                    reads=["Ub"] + [("gdec", par, h) for h in range(4)], writes=[("Sst", sn)])
                            add("act", lambda e, sn=sn: e.activation(Sbf[sn], Sst[sn], AF.Copy),
                                reads=[("Sst", sn)], writes=[("Sbf", sn)])
                        for f_ in flush_now:
                            f_()
                        add("act", lambda e, r0=r0, r1=r1, ob=ob: e.activation(
                            osq[r0:r1, :, :], ps_t[r0:r1, ob, :].rearrange("p (h v) -> p h v", h=4), AF.Square),
                            reads=[("ps", ob)], writes=[("osq", ce)])
                        add("dve", lambda e, r0=r0, r1=r1: e.reduce_sum(ssD[r0:r1, 0, :], osq[r0:r1, :, :], AX.X),
                            reads=[("osq", ce)], writes=[("ssD", 0, ce)])
                        add("act", lambda e, r0=r0, r1=r1: e.activation(ssD[r0:r1, 1, :], ssD[r0:r1, 0, :], AF.Ln,
                                                                        bias=EPS, scale=1.0 / 128),
                            reads=[("ssD", 0, ce)], writes=[("ssD", 1, ce)])
                        add("act", lambda e, r0=r0, r1=r1: e.activation(ssD[r0:r1, 2, :], ssD[r0:r1, 1, :], AF.Exp, scale=-0.5),
                            reads=[("ssD", 1, ce)], writes=[("ssD", 2, ce)])
                        add("dve", lambda e, r0=r0, r1=r1, ob=ob: e.tensor_tensor(
                            onb[r0:r1, :, :], ps_t[r0:r1, ob, :].rearrange("p (h v) -> p h v", h=4),
                            ssD[r0:r1, 2, :].unsqueeze(2).to_broadcast([64, 4, 128]), ALU.mult),
                            reads=[("ps", ob), ("ssD", 2, ce)], writes=[("onb", ce)])
                        def d_tail(r0=r0, r1=r1, ce=ce, par=par, tok0=tok0, tc0=tc0, tt=tt, c=c):
                            ts_ = rot("tp7", 2)

                            def tro(e, r0=r0, r1=r1, ts_=ts_):
                                ins = None
                                for h in range(4):
                                    ins = e.transpose(psb(7)[:, ts_ * 256 + h * 64:ts_ * 256 + (h + 1) * 64],
                                                      onb[r0:r1, h, :], ident[r0:r1, r0:r1])
                                return ins
                            add("pe", tro, reads=[("onb", ce), "ident"], writes=[("ps", 7)])
                            add("dve", lambda e, ts_=ts_, par=par, tok0=tok0, tc0=tc0: e.scalar_tensor_tensor(
                                ocatT[:, 4:8, tok0 + tc0:tok0 + tc0 + 64],
                                psb(7)[:, ts_ * 256:(ts_ + 1) * 256].rearrange("p (h t) -> p h t", h=4),
                                hgw[:, 0:1], Gt[par][:, :, tc0:tc0 + 64], ALU.mult, ALU.mult),
                                reads=[("ps", 7), "hgw"] + [("Gt", par, h) for h in range(4)],
                                writes=[("ocatT", 4, tt, c)])
                        dpend.append(d_tail)
            for f_ in dpend:
                f_()
            del dpend[:]
            if b == 0:
                tap("ocatT", ocatT, [("ocatT", 4, tt, c) for tt in range(8) for c in range(4)] +
                    [("ocatT", h, u) for h in range(4) for u in range(8)])

            mark("D%d" % b)
            S.barrier()
            for i in range(NT):
                xs = rot("xt", 3)
                add("sp", lambda e, xs=xs, i=i, b=b: e.dma_start(out=xt[xs], in_=x[b, i * 128:(i + 1) * 128, :]),
                    writes=[("xt", xs)], chan=("xt", xs))
                pp = (0, 3)[rot("pp", 2)]

                def oproj(e, i=i, pp=pp):
                    ins = None
                    for half in range(2):
                        for kc in range(8):
                            ins = e.matmul(ps(pp + half), ocatT[:, kc, i * 128:(i + 1) * 128],
                                           wout[:, kc, half * 512:(half + 1) * 512],
                                           start=(kc == 0), stop=(kc == 7))
                    return ins
                add("pe", oproj, reads=["wout"], writes=[("ps", pp), ("ps", pp + 1)])
                x1s = rot("x1t", 2)
                add("dve", lambda e, xs=xs, x1s=x1s, pp=pp: e.tensor_tensor(
                    x1t[x1s].rearrange("p (a n) -> p a n", a=2), xt[xs].rearrange("p (a n) -> p a n", a=2),
                    ps_t[:, pp:pp + 2, :], ALU.add),
                    reads=[("xt", xs), ("ps", pp), ("ps", pp + 1)], writes=[("x1t", x1s)])
                si = rot("st", 8)
                ss, ln_, rs = stat(si)
                gi = b * NT + i
                add("act", lambda e, x1s=x1s, ss=ss: e.activation(junk, x1t[x1s], AF.Square, accum_out=ss),
                    reads=[("x1t", x1s)], writes=["junk", ("ss", si)])
                add("act", lambda e, ss=ss, ln_=ln_: e.activation(ln_, ss, AF.Ln, bias=EPS, scale=1.0 / D),
                    reads=[("ss", si)], writes=[("ln", si)])
                add("act", lambda e, gi=gi, ln_=ln_: e.activation(rstd2[:, gi:gi + 1], ln_, AF.Exp, scale=-0.5),
                    reads=[("ln", si)], writes=[("rstd2", gi)])
                add("sp", lambda e, x1s=x1s, i=i, b=b: e.dma_start(out=out[b, i * 128:(i + 1) * 128, :], in_=x1t[x1s]),
                    reads=[("x1t", x1s)], chan=("x1st", x1s))
                if b == 0 and i == 0:
                    tap("x1", x1t[x1s], [("x1t", x1s)])
            S.barrier()
            mark("E%d" % b)

        mark("P1")
        HC = NFC // 2
        WB = [0, 3, 11, 16, NFC]

        def wpiece(jc):
            for p_ in range(4):
                if WB[p_] <= jc < WB[p_ + 1]:
                    return p_

        def ld_wup(p_):
            c0, c1 = WB[p_] * 128, WB[p_ + 1] * 128
            for part in range(2):
                add("pool", lambda e, c0=c0, c1=c1, part=part: e.dma_start(
                    out=wup[:, :, part * FF + c0:part * FF + c1],
                    in_=w_up[:, part * FF + c0:part * FF + c1].rearrange("(k p) n -> p k n", p=128)),
                    writes=[("wup", p_, part)], chan=("wup", p_, part))

        def ld_wdown(hf_):
            add("pool", lambda e, hf_=hf_: e.dma_start(
                out=wdown[:, hf_ * HC:(hf_ + 1) * HC, :],
                in_=w_down[hf_ * HC * 128:(hf_ + 1) * HC * 128, :].rearrange("(c p) n -> p c n", p=128)),
                writes=[("wdown", hf_)], chan=("wdown", hf_))

        ld_wup(0); ld_wup(1); ld_wdown(0); ld_wup(2); ld_wup(3); ld_wdown(1)
        mark("W2")
        def p2_front(t):
            b, j = divmod(t, 8)
            xs = t % 2
            gi0 = b * NT + 2 * j
            add("sp", lambda e, xs=xs, j=j, b=b: e.dma_start(
                out=x1p[xs], in_=out[b, j * 256:(j + 1) * 256, :].rearrange("(s p) d -> p s d", p=128)),
                writes=[("x1p", xs)], chan=("x1ld", xs))
            for s_ in range(2):
                add("dve", lambda e, xs=xs, s_=s_, gi0=gi0: e.tensor_scalar(
                    h2n[xs][:, s_, :], x1p[xs][:, s_, :], rstd2[:, gi0 + s_:gi0 + s_ + 1], None, ALU.mult),
                    reads=[("x1p", xs)], writes=[("h2n", xs, s_)])
            hx = xs
            for kh in range(2):
                tb = 2

                def trh(e, xs=xs, kh=kh, tb=tb):
                    ins = None
                    for kq in range(4):
                        kc = kh * 4 + kq
                        for s_ in range(2):
                            ins = e.transpose(psb(tb)[:, kq * 256 + s_ * 128:kq * 256 + (s_ + 1) * 128],
                                              h2n[xs][:, s_, kc * 128:(kc + 1) * 128], ident[:, :])
                    return ins
                add("pe", trh, reads=[("h2n", xs, 0), ("h2n", xs, 1)], writes=[("ps", tb)])
                add("dve", lambda e, hx=hx, kh=kh, tb=tb: e.tensor_tensor(
                    h2T[hx][:, kh * 4:(kh + 1) * 4, :],
                    psb(tb)[:, 0:1024].rearrange("p (k t) -> p k t", k=4),
                    ln2c[:, kh * 4:(kh + 1) * 4].unsqueeze(2).to_broadcast([128, 4, 256]), ALU.mult),
                    reads=[("ps", tb)], writes=[("h2T", hx, kh)])

        p2_front(0)
        for b in range(NB):
            for j in range(8):
                t_ = b * 8 + j
                xs = t_ % 2
                hx = xs
                gk_of = {}

                def emit_down(jc, gk_of=gk_of):
                    gk = gk_of[jc]

                    def dmm(e, jc=jc, gk=gk):
                        ins = None
                        for s in range(2):
                            for half in range(2):
                                ins = e.matmul(ps(4 + 2 * s + half), gbuf[gk][:, s * 128:(s + 1) * 128],
                                               wdown[:, jc, half * 512:(half + 1) * 512],
                                               start=(jc == 0), stop=(jc == NFC - 1))
                        return ins
                    add("pe", dmm, reads=[("g", gk), ("wdown", jc // HC)], writes=[("ps", 4), ("ps", 5), ("ps", 6), ("ps", 7)])

                for jc in range(NFC):
                    ub = (0, 1, 3)[rot("uv", 3)]

                    def upmm(e, jc=jc, ub=ub, hx=hx):
                        ins = None
                        for part in range(2):
                            for kc in range(8):
                                ins = e.matmul(ps(ub)[:, part * 256:(part + 1) * 256],
                                               wup[:, kc, part * FF + jc * 128:part * FF + (jc + 1) * 128],
                                               h2T[hx][:, kc, :], start=(kc == 0), stop=(kc == 7))
                        return ins
                    add("pe", upmm, reads=[("wup", wpiece(jc), 0), ("wup", wpiece(jc), 1), ("h2T", hx, 0), ("h2T", hx, 1)],
                        writes=[("ps", ub)])
                    ai = rot("acc2", 3)
                    a = accb[ai]
                    add("act", lambda e, a=a, ub=ub, jc=jc: e.activation(
                        a, ps(ub)[:, 0:256], AF.Identity, scale=cw[:, 2, jc:jc + 1], bias=cb[:, jc:jc + 1]),
                        reads=[("ps", ub)], writes=[("acc2", ai)])
                    add("dve", lambda e, a=a, ub=ub, jc=jc: e.scalar_tensor_tensor(
                        a[:, 1:256], ps(ub)[:, 0:255], cw[:, 1, jc:jc + 1], a[:, 1:256], ALU.mult, ALU.add),
                        reads=[("ps", ub), ("acc2", ai)], writes=[("acc2", ai)])
                    add("dve", lambda e, a=a, ub=ub, jc=jc: e.scalar_tensor_tensor(
                        a[:, 2:256], ps(ub)[:, 0:254], cw[:, 0, jc:jc + 1], a[:, 2:256], ALU.mult, ALU.add),
                        reads=[("ps", ub), ("acc2", ai)], writes=[("acc2", ai)])
                    if j > 0:
                        add("dve", lambda e, a=a, jc=jc: e.scalar_tensor_tensor(
                            a[:, 0:1], halo[:, jc, 1:2], cw[:, 1, jc:jc + 1], a[:, 0:1], ALU.mult, ALU.add),
                            reads=[("halo", jc), ("acc2", ai)], writes=[("acc2", ai)])
                        add("dve", lambda e, a=a, jc=jc: e.scalar_tensor_tensor(
                            a[:, 0:2], halo[:, jc, 0:2], cw[:, 0, jc:jc + 1], a[:, 0:2], ALU.mult, ALU.add),
                            reads=[("halo", jc), ("acc2", ai)], writes=[("acc2", ai)])
                    add("act", lambda e, ub=ub, jc=jc: e.activation(halo[:, jc, :], ps(ub)[:, 254:256], AF.Copy),
                        reads=[("ps", ub)], writes=[("halo", jc)])
                    sk = rot("sl", 2)
                    add("act", lambda e, a=a, sk=sk: e.activation(slb[sk], a, AF.Silu),
                        reads=[("acc2", ai)], writes=[("sl", sk)])
                    gk = rot("g", 4)
                    gk_of[jc] = gk
                    add("dve", lambda e, sk=sk, gk=gk, ub=ub: e.tensor_tensor(
                        gbuf[gk], slb[sk], ps(ub)[:, 256:512], ALU.mult),
                        reads=[("sl", sk), ("ps", ub)], writes=[("g", gk)])
                    if jc >= 2:
                        emit_down(jc - 2)
                    if jc == 9 and t_ + 1 < NB * 8:
                        p2_front(t_ + 1)
                emit_down(NFC - 2)
                emit_down(NFC - 1)
                for s in range(2):
                    add("dve", lambda e, xs=xs, s=s: e.tensor_tensor(
                        x1p[xs][:, s, :].rearrange("p (a n) -> p a n", a=2),
                        x1p[xs][:, s, :].rearrange("p (a n) -> p a n", a=2),
                        ps_t[:, 4 + 2 * s:6 + 2 * s, :], ALU.add),
                        reads=[("x1p", xs), ("ps", 4 + 2 * s), ("ps", 5 + 2 * s)], writes=[("x1p", xs)])
                add("sp", lambda e, xs=xs, j=j, b=b: e.dma_start(
                    out=out[b, j * 256:(j + 1) * 256, :].rearrange("(s p) d -> p s d", p=128), in_=x1p[xs]),
                    reads=[("x1p", xs)], chan=("ost", xs))
        mark("END")
        lim = None
        if limit_mark is not None:
            lim = marks[limit_mark]
        if marks_out is not None:
            marks_out.update(marks)
        S.emit(limit=lim)
    return nc


_PARAM_NAMES = ["ln1_w", "w_in", "q_norm_w", "k_norm_w", "lam_q1", "lam_k1", "lam_q2", "lam_k2",
                "diff_subln_w", "hgrn_lb_logits", "hgrn_norm_w", "w_out", "ln2_w", "w_up", "conv_w",
                "conv_b", "w_down"]


def _core_inputs(inputs, core):
    m = {"x": np.ascontiguousarray(inputs["x"][core * NB:(core + 1) * NB], dtype=np.float32)}
    for n in _PARAM_NAMES:
        a = np.asarray(inputs[n], dtype=np.float32)
        if n == "hgrn_lb_logits":
            m[n] = np.ascontiguousarray(a)
        elif n in ("w_in", "w_out", "w_up", "w_down", "conv_w"):
            m[n] = np.ascontiguousarray(a[0])
        else:
            m[n] = np.ascontiguousarray(a.reshape(1, -1))
    return m


def kernel(**inputs):
    nc = build_program()
    in_maps = [_core_inputs(inputs, c) for c in range(NCORES)]
    res = run_bass_kernel_spmd(nc, in_maps, core_ids=list(range(NCORES)))
    outs = [np.asarray(r["out"], dtype=np.float32) for r in res.results]
    return np.concatenate(outs, axis=0)
```

```python
import contextlib
import math

import numpy as np
import concourse.bass as bass
import concourse.mybir as mybir
from concourse.bass_utils import run_bass_kernel_spmd

F32 = mybir.dt.float32
BF16 = mybir.dt.bfloat16
ALU = mybir.AluOpType
AF = mybir.ActivationFunctionType
AX = mybir.AxisListType

NCORES = 8
NB = 2
SEQ = 2048
D = 1024
NT = SEQ // 128
FF = 2816
NFC = FF // 128
INC = 3584
EPS = 1e-6
LAM_INIT = 0.8 - 0.6 * math.exp(-0.3 * 0)
ENGS = ("pe", "act", "dve", "pool", "sp")


class _Op:
    __slots__ = ("eng", "fn", "deps", "signals", "sig", "chan", "chan_val")


class Sched:
    def __init__(self, nc):
        self.nc = nc
        self.ops = []
        self.eng_ops = {e: [] for e in ENGS}
        self.last_writer = {}
        self.readers = {}
        self.chan_count = {}
        self.chan_last = {}
        self.eng_last = {}
        self.bar = set()

    def add(self, eng, fn, reads=(), writes=(), chan=None):
        op = _Op()
        op.eng = eng; op.fn = fn; op.chan = chan
        op.signals = False; op.sig = None; op.chan_val = None
        writes = list(writes) + [k for k in reads if isinstance(k, tuple) and k[0] == "ps"]
        deps = set(self.bar)
        for k in reads:
            w = self.last_writer.get(k)
            if w is not None:
                deps.add(w)
        for k in writes:
            w = self.last_writer.get(k)
            if w is not None:
                deps.add(w)
            for r in self.readers.get(k, ()):
                deps.add(r)
        for k in reads:
            self.readers.setdefault(k, []).append(op)
        for k in writes:
            self.last_writer[k] = op
            self.readers[k] = []
        deps.discard(op)
        if eng == "pe":
            deps = {d for d in deps if not (d.eng == "pe" and d.chan is None)}
        op.deps = deps
        if chan is not None:
            self.chan_count[chan] = self.chan_count.get(chan, 0) + 16
            op.chan_val = self.chan_count[chan]
            self.chan_last[chan] = op
        else:
            self.eng_last[eng] = op
        self.ops.append(op)
        self.eng_ops[eng].append(op)
        return op

    def barrier(self):
        self.bar = set(self.eng_last.values()) | set(self.chan_last.values())
        self.last_writer = {}
        self.readers = {}

    def emit(self, final_wait_eng="sp", limit=None):
        nc = self.nc
        if limit is not None:
            kept = self.ops[:limit]
            self.ops = kept
            ks = set(kept)
            self.eng_ops = {e: [o for o in self.eng_ops[e] if o in ks] for e in ENGS}
            self.chan_count = {}
            for o in kept:
                if o.chan is not None:
                    self.chan_count[o.chan] = max(self.chan_count.get(o.chan, 0), o.chan_val)
        for op in self.ops:
            for d in op.deps:
                if d.chan is None:
                    d.signals = True
        cnt = {e: 0 for e in ENGS}
        for op in self.ops:
            if op.chan is None and op.signals:
                cnt[op.eng] += 1
                op.sig = cnt[op.eng]
        with contextlib.ExitStack() as st:
            esem = {e: st.enter_context(nc.semaphore("s_" + e)) for e in ENGS}
            csem = {}
            for i, c in enumerate(self.chan_count):
                csem[c] = st.enter_context(nc.semaphore("c%d" % i))
            block = st.enter_context(nc.Block())
            handles = {"pe": block.tensor, "act": block.scalar, "dve": block.vector,
                       "pool": block.gpsimd, "sp": block.sync}
            for e in ENGS:
                ops = self.eng_ops[e]
                if not ops and e != final_wait_eng:
                    continue

                def body(eng, ops=ops, e=e):
                    known = {}
                    for op in ops:
                        need = {}
                        for d in op.deps:
                            if d.chan is not None:
                                key = ("c", d.chan); val = d.chan_val
                            else:
                                key = ("e", d.eng); val = d.sig
                            if val > need.get(key, 0):
                                need[key] = val
                        for key, val in need.items():
                            if known.get(key, 0) >= val:
                                continue
                            known[key] = val
                            sem = csem[key[1]] if key[0] == "c" else esem[key[1]]
                            eng.wait_ge(sem, val)
                        ins = op.fn(eng)
                        if op.chan is not None:
                            ins.then_inc(csem[op.chan], 16)
                        elif op.signals:
                            ins.then_inc(esem[e], 1)
                    if e == final_wait_eng:
                        for c, v in self.chan_count.items():
                            if known.get(("c", c), 0) < v:
                                eng.wait_ge(csem[c], v)

                handles[e](body)


class Arena:
    def __init__(self, ap_f32, nwords):
        self.ap = ap_f32
        self.n = nwords
        self.off = 0

    def alloc(self, free_shape, dtype):
        n = 1
        for s in free_shape:
            n *= s
        nbytes = n * (2 if dtype == BF16 else 4)
        nw = (nbytes + 3) // 4
        nw = (nw + 7) // 8 * 8
        assert self.off + nw <= self.n, ("arena overflow", self.off, nw, self.n)
        v = self.ap[:, self.off:self.off + nw]
        self.off += nw
        if dtype == BF16:
            v = v.bitcast(BF16)
        v = v[:, 0:n]
        if len(free_shape) == 2:
            v = v.rearrange("p (a b) -> p a b", a=free_shape[0])
        elif len(free_shape) == 3:
            v = v.rearrange("p (a b c) -> p a b c", a=free_shape[0], b=free_shape[1])
        return v


def build_program(taps=None, limit_mark=None, marks_out=None):
    nc = bass.Bass("TRN2", target_bir_lowering=False)

    def din(name, shape):
        return nc.dram_tensor(name, shape, F32, kind="ExternalInput").ap()

    x = din("x", [NB, SEQ, D])
    ln1_w = din("ln1_w", [1, D])
    w_in = din("w_in", [D, INC])
    q_norm_w = din("q_norm_w", [1, 64])
    k_norm_w = din("k_norm_w", [1, 64])
    lam_q1 = din("lam_q1", [1, 64])
    lam_k1 = din("lam_k1", [1, 64])
    lam_q2 = din("lam_q2", [1, 64])
    lam_k2 = din("lam_k2", [1, 64])
    diff_subln_w = din("diff_subln_w", [1, 128])
    hgrn_lb_logits = din("hgrn_lb_logits", [2, 512])
    hgrn_norm_w = din("hgrn_norm_w", [1, 128])
    w_out = din("w_out", [D, D])
    ln2_w = din("ln2_w", [1, D])
    w_up = din("w_up", [D, 2 * FF])
    conv_w = din("conv_w", [3, FF])
    conv_b = din("conv_b", [1, FF])
    w_down = din("w_down", [FF, D])
    out = nc.dram_tensor("out", [NB, SEQ, D], F32, kind="ExternalOutput").ap()
    tap_t = {}
    if taps:
        for name, (shape, dt_) in taps.items():
            tap_t[name] = nc.dram_tensor("tap_" + name, shape, dt_, kind="ExternalOutput").ap()

    es = contextlib.ExitStack()
    with es:
        def sb(name, shape, dt_):
            return es.enter_context(nc.sbuf_tensor(name, shape, dt_))

        identf = sb("identf", [128, 128], F32)
        ident = sb("ident", [128, 128], BF16)
        blockones = sb("blockones", [128, 128], BF16)
        mask2 = sb("mask2", [128, 64], F32)
        onescol = sb("onescol", [128, 1], F32)
        NSTG = 116
        stage = sb("stage", [128, 128], F32)
        cst = sb("cst", [128, NSTG], F32)
        ln1c = cst[:, 0:8]
        ln2c = cst[:, 8:16]
        cw = cst[:, 16:82].rearrange("p (j c) -> p j c", j=3)
        cb = cst[:, 82:104]
        lbl = cst[:, 104:112].rearrange("p (r h) -> p r h", r=2)
        hgw = cst[:, 115:116]
        gqk = sb("gqk", [128, 2], F32)
        subw = sb("subw", [128, 1], F32)
        lamv = sb("lamv", [128, 4, 64], F32)
        lamp = sb("lamp", [128, 2, 64], F32)
        lams = sb("lams", [128, 2], F32)
        neglam = sb("neglam", [128, 1], F32)
        lbt = sb("lbt", [128, 4], F32)
        lb = sb("lb", [128, 4], F32)
        ln1mlb = sb("ln1mlb", [128, 4], F32)
        rstd2 = sb("rstd2", [128, NB * NT], F32)
        st4 = sb("st4", [128, 3, 8], F32)
        carry = sb("carry", [128, 4], F32)

        nwords = nc.sbuf_bytes_remaining // 4 - 64
        arena_t = sb("arena", [128, nwords], F32)
        ps_t = es.enter_context(nc.psum_tensor("ps", [128, 8, 512], F32))

        def ps(bank):
            return ps_t[:, bank, :]

        def psb(bank):
            return ps_t[:, bank, :].bitcast(BF16)

        S = Sched(nc)
        add = S.add
        cnt = {}
        marks = {}

        def mark(name):
            marks[name] = len(S.ops)

        def rot(name, n):
            v = cnt.get(name, 0)
            cnt[name] = v + 1
            return v % n

        def tap(name, src_ap, reads):
            if name in tap_t:
                add("sp", lambda e: e.dma_start(out=tap_t[name], in_=src_ap), reads=reads,
                    chan=("tap", name))

        def ld_small(dst, src, key):
            add("sp", lambda e: e.dma_start(out=dst, in_=src), writes=[key], chan=("c", key))

        add("pool", lambda e: e.memset(stage[:, :], 0.0), writes=["stage0"])
        srows = [
            (0, 8, ln1_w.rearrange("o (k p) -> (o k) p", p=128)),
            (8, 8, ln2_w.rearrange("o (k p) -> (o k) p", p=128)),
            (16, 66, conv_w.rearrange("j (c p) -> (j c) p", p=128)),
            (82, 22, conv_b.rearrange("o (c p) -> (o c) p", p=128)),
            (104, 8, hgrn_lb_logits.rearrange("r (h k) -> (r h) k", k=128)),
            (114, 1, diff_subln_w),
            (115, 1, hgrn_norm_w),
        ]
        skeys = ["stage0"]
        for r0_, n_, src_ in srows:
            add("sp", lambda e, r0_=r0_, n_=n_, src_=src_: e.dma_start(out=stage[r0_:r0_ + n_, :], in_=src_),
                reads=["stage0"], writes=[("stage", r0_)], chan=("c", "stage", r0_))
            skeys.append(("stage", r0_))
        for c in range(2):
            for r0_, src_ in ((112, q_norm_w), (113, k_norm_w)):
                add("sp", lambda e, r0_=r0_, src_=src_, c=c: e.dma_start(
                    out=stage[r0_:r0_ + 1, c * 64:(c + 1) * 64], in_=src_),
                    reads=["stage0"], writes=[("stage", r0_, c)], chan=("c", "stage", r0_, c))
                skeys.append(("stage", r0_, c))
        for i, lv in enumerate((lam_q1, lam_k1, lam_q2, lam_k2)):
            ld_small(lamv[:, i, :], lv.partition_broadcast(128), ("lamv", i))

        add("pool", lambda e: e.memset(identf[:, :], 1.0), writes=["identf"])
        add("pool", lambda e: e.affine_select(out=identf[:, :], in_=identf[:, :], pattern=[[-1, 128]],
                                              compare_op=ALU.is_equal, fill=0.0, base=0,
                                              channel_multiplier=1),
            reads=["identf"], writes=["identf"])
        add("dve", lambda e: e.tensor_copy(ident[:, :], identf[:, :]), reads=["identf"], writes=["ident"])
        add("pe", lambda e: e.transpose(ps_t[:, 0, 0:NSTG], stage[0:NSTG, :], identf[0:NSTG, 0:NSTG]),
            reads=skeys + ["identf"], writes=[("ps", 0)])
        add("dve", lambda e: e.tensor_copy(cst[:, :], ps_t[:, 0, 0:NSTG]), reads=[("ps", 0)],
            writes=["ln1c", "ln2c", "cw", "cb", "lbl", "cst"])
        add("pool", lambda e: e.memset(mask2[:, :], 1.0), writes=["mask2"])
        for hf_ in range(2):
            add("pool", lambda e, hf_=hf_: e.affine_select(
                out=mask2[hf_ * 64:(hf_ + 1) * 64, :], in_=mask2[hf_ * 64:(hf_ + 1) * 64, :],
                pattern=[[1, 64]], compare_op=ALU.is_ge, fill=0.0, base=0, channel_multiplier=-1),
                reads=["mask2"], writes=["mask2"])
        add("dve", lambda e: e.memset(blockones[:, :], 0.0), writes=["blockones"])
        for hf_ in range(2):
            add("dve", lambda e, hf_=hf_: e.memset(
                blockones[hf_ * 64:(hf_ + 1) * 64, hf_ * 64:(hf_ + 1) * 64], 1.0 / 64.0),
                writes=["blockones"])
        add("pool", lambda e: e.memset(onescol[:, :], 1.0), writes=["onescol"])
        add("dve", lambda e: e.tensor_scalar(gqk[:, 0:1], cst[:, 112:113], 0.125, None, ALU.mult),
            reads=["cst"], writes=["gqk"])
        add("dve", lambda e: e.tensor_copy(gqk[:, 1:2], cst[:, 113:114]), reads=["cst"], writes=["gqk"])
        add("dve", lambda e: e.tensor_scalar(subw[:, :], cst[:, 114:115], 1.0 - LAM_INIT, None, ALU.mult),
            reads=["cst"], writes=["subw"])
        add("dve", lambda e: e.tensor_tensor(lamp[:, 0, :], lamv[:, 0, :], lamv[:, 1, :], ALU.mult),
            reads=[("lamv", 0), ("lamv", 1)], writes=[("lamp", 0)])
        add("dve", lambda e: e.tensor_tensor(lamp[:, 1, :], lamv[:, 2, :], lamv[:, 3, :], ALU.mult),
            reads=[("lamv", 2), ("lamv", 3)], writes=[("lamp", 1)])
        add("dve", lambda e: e.reduce_sum(lams[:, :], lamp[:, :, :], AX.X),
            reads=[("lamp", 0), ("lamp", 1)], writes=["lams"])
        add("act", lambda e: e.activation(lams[:, :], lams[:, :], AF.Exp), reads=["lams"], writes=["lams"])
        add("dve", lambda e: e.tensor_tensor(neglam[:, :], lams[:, 1:2], lams[:, 0:1], ALU.subtract),
            reads=["lams"], writes=["neglam"])
        add("dve", lambda e: e.tensor_scalar(neglam[:, :], neglam[:, :], -LAM_INIT, None, ALU.add),
            reads=["neglam"], writes=["neglam"])
        add("dve", lambda e: e.tensor_tensor(lbt[:, :], lbl[:, 1, :], lbl[:, 0, :], ALU.subtract),
            reads=["lbl"], writes=["lbt"])
        add("act", lambda e: e.activation(lbt[:, :], lbt[:, :], AF.Exp), reads=["lbt"], writes=["lbt"])
        add("dve", lambda e: e.tensor_scalar(lbt[:, :], lbt[:, :], 1.0, None, ALU.add),
            reads=["lbt"], writes=["lbt"])
        add("dve", lambda e: e.reciprocal(lb[:, :], lbt[:, :]), reads=["lbt"], writes=["lb"])
        add("act", lambda e: e.activation(ln1mlb[:, :], lb[:, :], AF.Ln, bias=1.0, scale=-1.0),
            reads=["lb"], writes=["ln1mlb"])

        mark("consts")
        A1 = Arena(arena_t[:, :], nwords)
        wout = A1.alloc([8, D], BF16)
        winb = [A1.alloc([8, 512], BF16) for _ in range(4)]
        hT = A1.alloc([8, SEQ], BF16)
        ocatT = A1.alloc([8, SEQ], BF16)
        xt = [A1.alloc([D], F32) for _ in range(3)]
        junk = A1.alloc([D], BF16)
        hn = [A1.alloc([D], BF16) for _ in range(2)]
        x1t = [A1.alloc([D], F32) for _ in range(2)]
        local_base = A1.off
        qkT = [A1.alloc([2, SEQ], BF16) for _ in range(2)]
        vaug = A1.alloc([NT, 2, 130], BF16)
        pt = [A1.alloc([2, 256], BF16) for _ in range(3)]
        pd = [A1.alloc([2, 256], BF16) for _ in range(2)]
        sq = [A1.alloc([512], BF16) for _ in range(2)]
        lnms = [A1.alloc([512], F32) for _ in range(2)]
        t0 = [A1.alloc([2, 128], F32) for _ in range(2)]
        t1a = [A1.alloc([2, 128], F32) for _ in range(2)]
        od = [A1.alloc([2, 128], F32) for _ in range(2)]
        odn = [A1.alloc([2, 128], BF16) for _ in range(2)]
        junk2 = A1.alloc([128], BF16)
        rz = [A1.alloc([4], F32) for _ in range(2)]
        rzl = [A1.alloc([2], F32) for _ in range(2)]
        attn_end = A1.off
        A1.off = local_base
        he = [A1.alloc([256], F32) for _ in range(2)]
        hL1 = [A1.alloc([256], F32) for _ in range(2)]
        hL2 = [A1.alloc([256], F32) for _ in range(2)]
        ht1 = [A1.alloc([256], F32) for _ in range(2)]
        heg = [A1.alloc([256], F32) for _ in range(2)]
        hBx = [A1.alloc([264], F32) for _ in range(2)]
        QtT = [A1.alloc([4, 256], BF16) for _ in range(2)]
        KtT = [A1.alloc([4, 256], BF16) for _ in range(2)]
        Gt = [A1.alloc([4, 256], BF16) for _ in range(2)]
        vtok = [A1.alloc([2, 512], BF16) for _ in range(2)]
        gdec = [A1.alloc([4, 4], F32) for _ in range(2)]
        Ktok = [A1.alloc([512], BF16) for _ in range(2)]
        AT = [A1.alloc([4, 64], BF16) for _ in range(2)]
        Ub = A1.alloc([4, 128], F32)
        Sst = [A1.alloc([4, 128], F32) for _ in range(2)]
        Sbf = [A1.alloc([4, 128], BF16) for _ in range(2)]
        osq = A1.alloc([4, 128], F32)
        onb = A1.alloc([4, 128], BF16)
        ssD = A1.alloc([3, 4], F32)
        A1.off = max(A1.off, attn_end)

        A2 = Arena(arena_t[:, :], nwords)
        wup = A2.alloc([8, 2 * FF], BF16)
        wdown = A2.alloc([NFC, D], BF16)
        x1p = [A2.alloc([2, D], F32) for _ in range(2)]
        h2n = [A2.alloc([2, D], BF16) for _ in range(2)]
        h2T = [A2.alloc([8, 256], BF16) for _ in range(2)]
        accb = [A2.alloc([256], F32) for _ in range(3)]
        slb = [A2.alloc([256], F32) for _ in range(2)]
        gbuf = [A2.alloc([256], BF16) for _ in range(4)]
        halo = A2.alloc([NFC, 2], F32)

        add("pool", lambda e: e.dma_start(out=wout, in_=w_out.rearrange("(k p) n -> p k n", p=128)),
            writes=["wout"], chan="wout")

        def load_win(g, slot):
            add("pool", lambda e: e.dma_start(
                out=winb[slot], in_=w_in[:, g * 512:(g + 1) * 512].rearrange("(k p) n -> p k n", p=128)),
                writes=[("win", slot)], chan=("win", slot))

        def stat(i):
            return st4[:, 0, i:i + 1], st4[:, 1, i:i + 1], st4[:, 2, i:i + 1]

        for b in range(NB):
            for g in range(4):
                load_win(g, g)
            for i in range(NT):
                xs = rot("xt", 3)
                add("sp", lambda e, xs=xs, i=i, b=b: e.dma_start(out=xt[xs], in_=x[b, i * 128:(i + 1) * 128, :]),
                    writes=[("xt", xs)], chan=("xt", xs))
                si = rot("st", 8)
                ss, ln_, rs = stat(si)
                add("act", lambda e, xs=xs, ss=ss: e.activation(junk, xt[xs], AF.Square, accum_out=ss),
                    reads=[("xt", xs)], writes=["junk", ("ss", si)])
                add("act", lambda e, ss=ss, ln_=ln_: e.activation(ln_, ss, AF.Ln, bias=EPS, scale=1.0 / D),
                    reads=[("ss", si)], writes=[("ln", si)])
                add("act", lambda e, rs=rs, ln_=ln_: e.activation(rs, ln_, AF.Exp, scale=-0.5),
                    reads=[("ln", si)], writes=[("rs", si)])
                hs = rot("hn", 2)
                add("dve", lambda e, hs=hs, xs=xs, rs=rs: e.tensor_scalar(hn[hs], xt[xs], rs, None, ALU.mult),
                    reads=[("xt", xs), ("rs", si)], writes=[("hn", hs)])
                tb = (2, 7)[rot("tpA", 2)]

                def tr8(e, hs=hs, tb=tb):
                    ins = None
                    for kc in range(8):
                        ins = e.transpose(psb(tb)[:, kc * 128:(kc + 1) * 128],
                                          hn[hs][:, kc * 128:(kc + 1) * 128], ident[:, :])
                    return ins
                add("pe", tr8, reads=[("hn", hs), "ident"], writes=[("ps", tb)])
                add("dve", lambda e, tb=tb, i=i: e.tensor_tensor(
                    hT[:, :, i * 128:(i + 1) * 128],
                    psb(tb)[:, 0:1024].rearrange("p (k t) -> p k t", k=8),
                    ln1c[:, :].unsqueeze(2).to_broadcast([128, 8, 128]), ALU.mult),
                    reads=[("ps", tb), "ln1c"], writes=[("hT", i)])
            mark("A%d" % b)
            if b == 0:
                tap("hT", hT, [("hT", i) for i in range(NT)])

            for hp in range(2):
                S.barrier()
                if hp == 0:
                    add("dve", lambda e: e.memset(vaug[:, :, :, 128:129], 1.0), writes=["vaug_ones"])
                    add("dve", lambda e: e.memset(vaug[:, :, :, 129:130], 0.0), writes=["vaug_ones"])
                    for k_ in range(2):
                        add("dve", lambda e, k_=k_: e.memset(pd[k_][64:128, :, 0:64], 0.0),
                            writes=[("pdz", k_)])
                items = [(T, hh, which) for T in range(4) for hh in range(2) for which in range(2)]
                st_ = {}

                def b_front(k):
                    T, hh, which = items[k]
                    h = 2 * hp + hh
                    pb = (0, 1, 5, 6)[rot("pa", 4)]
                    j = rot("sq", 2)
                    st_[k] = (pb, j)

                    def proj(e, which=which, h=h, T=T, pb=pb):
                        ins = None
                        for kc in range(8):
                            ins = e.matmul(ps(pb), winb[which][:, kc, h * 128:(h + 1) * 128],
                                           hT[:, kc, T * 512:(T + 1) * 512],
                                           start=(kc == 0), stop=(kc == 7))
                        return ins
                    add("pe", proj, reads=[("win", which)] + [("hT", 4 * T + jj) for jj in range(4)],
                        writes=[("ps", pb)])
                    add("act", lambda e, j=j, pb=pb: e.activation(sq[j], ps(pb), AF.Square),
                        reads=[("ps", pb)], writes=[("sq", j)])

                def b_back(k):
                    T, hh, which = items[k]
                    pb, j = st_[k]
                    mb = (3, 4)[rot("ms", 2)]
                    add("pe", lambda e, j=j, mb=mb: e.matmul(ps(mb), blockones[:, :], sq[j],
                                                             start=True, stop=True),
                        reads=[("sq", j), "blockones"], writes=[("ps", mb)])
                    add("act", lambda e, j=j, mb=mb: e.activation(lnms[j], ps(mb), AF.Ln, bias=EPS),
                        reads=[("ps", mb)], writes=[("lnms", j)])
                    add("act", lambda e, j=j: e.activation(lnms[j], lnms[j], AF.Exp, scale=-0.5),
                        reads=[("lnms", j)], writes=[("lnms", j)])
                    add("dve", lambda e, j=j, pb=pb, which=which, hh=hh, T=T: e.scalar_tensor_tensor(
                        qkT[which][:, hh, T * 512:(T + 1) * 512], ps(pb), gqk[:, which:which + 1],
                        lnms[j], ALU.mult, ALU.mult),
                        reads=[("ps", pb), ("lnms", j), "gqk"], writes=[("qkT", which, hh, T)])

                for k in range(len(items)):
                    b_front(k)
                    if k >= 1:
                        b_back(k - 1)
                b_back(len(items) - 1)
                for i in range(NT):
                    pb = (0, 1, 5, 6)[rot("pa", 4)]

                    def projv(e, i=i, pb=pb, hp=hp):
                        ins = None
                        for kc in range(8):
                            ins = e.matmul(ps(pb)[:, 0:256], hT[:, kc, i * 128:(i + 1) * 128],
                                           winb[2][:, kc, hp * 256:(hp + 1) * 256],
                                           start=(kc == 0), stop=(kc == 7))
                        return ins
                    add("pe", projv, reads=[("win", 2), ("hT", i)], writes=[("ps", pb)])
                    add("act", lambda e, i=i, pb=pb: e.activation(
                        vaug[:, i, :, 0:128], ps(pb)[:, 0:256].rearrange("p (h e) -> p h e", h=2), AF.Copy),
                        reads=[("ps", pb)], writes=[("vaug", i)])
                mark("B%d_%d" % (b, hp))
                if b == 0 and hp == 0:
                    tap("qT", qkT[0], [("qkT", 0, hh, T) for hh in range(2) for T in range(4)])
                    tap("kT", qkT[1], [("qkT", 1, hh, T) for hh in range(2) for T in range(4)])
                    tap("vaug", vaug, [("vaug", i) for i in range(NT)] + ["vaug_ones"])
                if hp == 1:
                    for g, slot in ((4, 0), (5, 1), (6, 2)):
                        load_win(g, slot)

                pending = []
                for hh in range(2):
                    h = 2 * hp + hh
                    for u in range(8):
                        ab0 = 5
                        ns = 2 * u + 2
                        info = {}

                        def emit_qk(i, hh=hh, u=u, info=info):
                            scb = (0, 3)[rot("sc", 2)]
                            lo = 128 if i == 2 * u + 1 else 0
                            n = 256 - lo
                            info[i] = (scb, lo, n)

                            def qk(e, hh=hh, u=u, i=i, scb=scb, lo=lo, n=n):
                                ins = None
                                for c in range(2):
                                    ins = e.matmul(ps(scb + c)[:, 0:n],
                                                   qkT[1][c * 64:(c + 1) * 64, hh, i * 128:(i + 1) * 128],
                                                   qkT[0][c * 64:(c + 1) * 64, hh, u * 256 + lo:(u + 1) * 256],
                                                   start=True, stop=True)
                                return ins
                            add("pe", qk, reads=[("qkT", 1, hh, i // 4), ("qkT", 0, hh, u // 2)],
                                writes=[("ps", scb), ("ps", scb + 1)])

                        def emit_exp_pv(i, hh=hh, u=u, info=info, ab0=ab0):
                            scb, lo, n = info[i]
                            scv = ps_t[:, scb:scb + 2, 0:256]
                            if i < 2 * u:
                                pi = rot("pt", 3)
                                pbuf = pt[pi]; pkey = ("pt", pi)
                                add("act", lambda e, pbuf=pbuf, scv=scv: e.activation(pbuf, scv, AF.Exp),
                                    reads=[("ps", scb), ("ps", scb + 1)], writes=[pkey])
                                extra = []
                            else:
                                pi = rot("pd", 2)
                                pbuf = pd[pi]; pkey = ("pd", pi)
                                add("act", lambda e, pbuf=pbuf, scv=scv, n=n: e.activation(
                                    pbuf[0:64, :, 0:n], scv[0:64, :, 0:n], AF.Exp),
                                    reads=[("ps", scb), ("ps", scb + 1)], writes=[pkey])
                                add("act", lambda e, pbuf=pbuf, scv=scv, n=n: e.activation(
                                    pbuf[64:128, :, 64:n], scv[64:128, :, 64:n], AF.Exp),
                                    reads=[("ps", scb), ("ps", scb + 1)], writes=[pkey])
                                extra = [("pdz", pi)]
                            return pbuf, pkey, extra

                        def emit_pv(i, pbuf, pkey, extra, hh=hh, u=u, ab0=ab0):
                            jbs = (1,) if i == 2 * u + 1 else (0, 1)

                            def pv(e, pbuf=pbuf, i=i, u=u, hh=hh, jbs=jbs, ab0=ab0):
                                ins = None
                                for jb in jbs:
                                    off = 0 if i == 2 * u + 1 else jb * 128
                                    for c in range(2):
                                        ins = e.matmul(ps(ab0 + jb)[:, c * 256:c * 256 + 130],
                                                       pbuf[:, c, off:off + 128], vaug[:, i, hh, :],
                                                       start=(i == 0 and c == 0), stop=(i == 2 * u + jb),
                                                       skip_group_check=True)
                                return ins
                            add("pe", pv, reads=[pkey, ("vaug", i), "vaug_ones"] + extra,
                                writes=[("ps", ab0 + jb) for jb in jbs])

                        emit_qk(0)
                        for i in range(ns):
                            pbuf, pkey, extra = emit_exp_pv(i)
                            if i + 1 < ns:
                                emit_qk(i + 1)
                            emit_pv(i, pbuf, pkey, extra)
                            if i == 0 and pending:
                                pending.pop(0)()
                        k_ = rot("fin", 2)
                        accv = ps_t[:, ab0:ab0 + 2, :].rearrange("p j (c w) -> p j c w", c=2)
                        rzv = rz[k_].rearrange("p (j c) -> p j c", j=2)
                        add("dve", lambda e, accv=accv, rzv=rzv: e.reciprocal(rzv, accv[:, :, :, 128]),
                            reads=[("ps", ab0), ("ps", ab0 + 1)], writes=[("rz", k_)])
                        add("dve", lambda e, rzv=rzv, k_=k_: e.tensor_scalar(
                            rzl[k_], rzv[:, :, 1], neglam[:, 0:1], None, ALU.mult),
                            reads=[("rz", k_), "neglam"], writes=[("rzl", k_)])
                        add("dve", lambda e, accv=accv, rzv=rzv, k_=k_: e.tensor_tensor(
                            t0[k_], accv[:, :, 0, 0:128], rzv[:, :, 0:1].to_broadcast([128, 2, 128]), ALU.mult),
                            reads=[("ps", ab0), ("ps", ab0 + 1), ("rz", k_)], writes=[("t0", k_)])
                        add("dve", lambda e, accv=accv, k_=k_: e.tensor_tensor(
                            t1a[k_], accv[:, :, 1, 0:128],
                            rzl[k_].unsqueeze(2).to_broadcast([128, 2, 128]), ALU.mult),
                            reads=[("ps", ab0), ("ps", ab0 + 1), ("rzl", k_)], writes=[("t1a", k_)])
                        add("pool", lambda e, k_=k_: e.tensor_tensor(od[k_], t0[k_], t1a[k_], ALU.add),
                            reads=[("t0", k_), ("t1a", k_)], writes=[("od", k_)])
                        si = rot("st", 8)
                        si2 = rot("st", 8)
                        assert si2 == si + 1
                        ssw = st4[:, 0, si:si + 2]; lnw = st4[:, 1, si:si + 2]; rsw = st4[:, 2, si:si + 2]
                        for jb in range(2):
                            add("act", lambda e, k_=k_, jb=jb, si=si: e.activation(
                                junk2, od[k_][:, jb, :], AF.Square, accum_out=st4[:, 0, si + jb:si + jb + 1]),
                                reads=[("od", k_)], writes=["junk2", ("ss", si + jb)])
                        add("act", lambda e, ssw=ssw, lnw=lnw: e.activation(lnw, ssw, AF.Ln, bias=EPS, scale=1.0 / 128),
                            reads=[("ss", si), ("ss", si + 1)], writes=[("ln", si), ("ln", si + 1)])
                        add("act", lambda e, rsw=rsw, lnw=lnw: e.activation(rsw, lnw, AF.Exp, scale=-0.5),
                            reads=[("ln", si), ("ln", si + 1)], writes=[("rs", si), ("rs", si + 1)])
                        add("dve", lambda e, k_=k_, rsw=rsw: e.tensor_tensor(
                            odn[k_], od[k_], rsw.unsqueeze(2).to_broadcast([128, 2, 128]), ALU.mult),
                            reads=[("od", k_), ("rs", si), ("rs", si + 1)], writes=[("odn", k_)])

                        def fin_tail(k_=k_, h=h, u=u, b=b, hp=hp, hh=hh):
                            tb = (2, 7)[rot("tpC", 2)]

                            def tr2(e, k_=k_, tb=tb):
                                ins = None
                                for jb in range(2):
                                    ins = e.transpose(psb(tb)[:, jb * 128:(jb + 1) * 128], odn[k_][:, jb, :], ident[:, :])
                                return ins
                            add("pe", tr2, reads=[("odn", k_), "ident"], writes=[("ps", tb)])
                            add("act", lambda e, tb=tb, h=h, u=u: e.activation(
                                ocatT[:, h, u * 256:(u + 1) * 256], psb(tb)[:, 0:256], AF.Identity, scale=subw[:, 0:1]),
                                reads=[("ps", tb), "subw"], writes=[("ocatT", h, u)])
                        pending.append(fin_tail)
                        mark("Cu_%d_%d_%d_%d" % (b, hp, hh, u))
                while pending:
                    pending.pop(0)()

            mark("C%d" % b)
            S.barrier()
            SL_HQ, SL_HF, SL_HI, SL_HG = 3, 0, 1, 2
            dpend = []
            for tt in range(8):
                par = tt % 2
                tok0 = tt * 256
                hkeys = [("hT", 2 * tt), ("hT", 2 * tt + 1)]
                for bi in range(2):
                    i = 2 * tt + bi
                    pb = (0, 1, 3)[rot("pd3", 3)]

                    def projhi(e, i=i, pb=pb):
                        ins = None
                        for kc in range(8):
                            ins = e.matmul(ps(pb), hT[:, kc, i * 128:(i + 1) * 128], winb[SL_HI][:, kc, :],
                                           start=(kc == 0), stop=(kc == 7))
                        return ins
                    add("pe", projhi, reads=[("win", SL_HI), ("hT", i)], writes=[("ps", pb)])
                    add("act", lambda e, par=par, bi=bi, pb=pb: e.activation(vtok[par][:, bi, :], ps(pb), AF.Copy),
                        reads=[("ps", pb)], writes=[("vtok", par, bi)])
                for h in range(4):
                    s_ = h % 2

                    def projf(slot, pb, h=h, tok0=tok0):
                        def f(e):
                            ins = None
                            for kc in range(8):
                                ins = e.matmul(ps(pb)[:, 0:256], winb[slot][:, kc, h * 128:(h + 1) * 128],
                                               hT[:, kc, tok0:tok0 + 256], start=(kc == 0), stop=(kc == 7))
                            return ins
                        return f
                    pb = (0, 1, 3)[rot("pd3", 3)]
                    add("pe", projf(SL_HF, pb), reads=[("win", SL_HF)] + hkeys, writes=[("ps", pb)])
                    add("act", lambda e, s_=s_, pb=pb: e.activation(he[s_], ps(pb)[:, 0:256], AF.Exp, scale=-1.0),
                        reads=[("ps", pb)], writes=[("he", s_)])
                    add("act", lambda e, s_=s_: e.activation(hL1[s_], he[s_], AF.Ln, bias=1.0),
                        reads=[("he", s_)], writes=[("hL1", s_)])
                    add("act", lambda e, s_=s_, h=h: e.activation(hL2[s_], he[s_], AF.Ln, bias=1.0, scale=lb[:, h:h + 1]),
                        reads=[("he", s_), "lb"], writes=[("hL2", s_)])
                    add("dve", lambda e, s_=s_, pb=pb: e.tensor_tensor(ht1[s_], ps(pb)[:, 0:256], hL1[s_], ALU.add),
                        reads=[("ps", pb), ("hL1", s_)], writes=[("ht1", s_)])
                    add("pool", lambda e, s_=s_: e.tensor_tensor(hL2[s_], hL2[s_], hL1[s_], ALU.subtract),
                        reads=[("hL2", s_), ("hL1", s_)], writes=[("hL2", s_)])
                    if tt == 0:
                        add("pool", lambda e, s_=s_: e.memset(hBx[s_][:, 0:1], 0.0), writes=[("hBx", s_)])
                    else:
                        add("pool", lambda e, s_=s_, h=h: e.tensor_copy(hBx[s_][:, 0:1], carry[:, h:h + 1]),
                            reads=[("carry", h)], writes=[("hBx", s_)])
                    add("dve", lambda e, s_=s_: e.tensor_tensor_scan(
                        hBx[s_][:, 1:257], onescol[:, 0:1].to_broadcast([128, 256]), hL2[s_],
                        hBx[s_][:, 0:1], ALU.mult, ALU.add),
                        reads=[("hL2", s_), ("hBx", s_), "onescol"], writes=[("hBx", s_)])
                    add("pool", lambda e, s_=s_, h=h: e.tensor_copy(carry[:, h:h + 1], hBx[s_][:, 256:257]),
                        reads=[("hBx", s_)], writes=[("carry", h)])
                    add("act", lambda e, s_=s_, h=h: e.activation(ht1[s_], ht1[s_], AF.Exp, scale=-1.0,
                                                                  bias=ln1mlb[:, h:h + 1]),
                        reads=[("ht1", s_), "ln1mlb"], writes=[("ht1", s_)])
                    add("dve", lambda e, s_=s_: e.tensor_tensor(
                        he[s_].rearrange("p (c t) -> p c t", c=4),
                        hBx[s_][:, 1:257].rearrange("p (c t) -> p c t", c=4),
                        hBx[s_][:, 0:256:64].unsqueeze(2).to_broadcast([128, 4, 64]), ALU.subtract),
                        reads=[("hBx", s_), ("he", s_), ("hL2", s_)], writes=[("he", s_)])
                    add("act", lambda e, s_=s_: e.activation(hL1[s_], he[s_], AF.Exp),
                        reads=[("he", s_), ("ht1", s_)], writes=[("hL1", s_)])
                    add("act", lambda e, s_=s_: e.activation(he[s_], he[s_], AF.Exp, scale=-1.0),
                        reads=[("he", s_), ("hL1", s_)], writes=[("he", s_)])
                    pb2 = (0, 1, 3)[rot("pd3", 3)]
                    add("pe", projf(SL_HQ, pb2), reads=[("win", SL_HQ)] + hkeys, writes=[("ps", pb2)])
                    add("dve", lambda e, s_=s_, pb2=pb2, par=par, h=h: e.tensor_tensor(
                        QtT[par][:, h, :], ps(pb2)[:, 0:256], hL1[s_], ALU.mult),
                        reads=[("ps", pb2), ("hL1", s_)], writes=[("QtT", par, h)])
                    add("pool", lambda e, s_=s_, par=par, h=h: e.tensor_tensor(
                        KtT[par][:, h, :], ht1[s_], he[s_], ALU.mult),
                        reads=[("ht1", s_), ("he", s_)], writes=[("KtT", par, h)])
                    add("pool", lambda e, s_=s_, par=par, h=h: e.tensor_copy(
                        gdec[par][:, h, :], hL1[s_].rearrange("p (c t) -> p c t", c=4)[:, :, 63]),
                        reads=[("hL1", s_)], writes=[("gdec", par, h)])
                    pb3 = (0, 1, 3)[rot("pd3", 3)]
                    add("pe", projf(SL_HG, pb3), reads=[("win", SL_HG)] + hkeys, writes=[("ps", pb3)])
                    add("act", lambda e, s_=s_, pb3=pb3: e.activation(heg[s_], ps(pb3)[:, 0:256], AF.Exp, scale=-1.0),
                        reads=[("ps", pb3)], writes=[("heg", s_)])
                    add("act", lambda e, s_=s_: e.activation(heg[s_], heg[s_], AF.Ln, bias=1.0),
                        reads=[("heg", s_)], writes=[("heg", s_)])
                    add("act", lambda e, s_=s_: e.activation(heg[s_], heg[s_], AF.Exp, scale=-1.0),
                        reads=[("heg", s_)], writes=[("heg", s_)])
                    add("dve", lambda e, s_=s_, pb3=pb3, par=par, h=h: e.tensor_tensor(
                        Gt[par][:, h, :], ps(pb3)[:, 0:256], heg[s_], ALU.mult),
                        reads=[("ps", pb3), ("heg", s_)], writes=[("Gt", par, h)])
                if b == 0 and tt == 0:
                    tap("QtT", QtT[0], [("QtT", 0, h) for h in range(4)])
                    tap("KtT", KtT[0], [("KtT", 0, h) for h in range(4)])
                    tap("Gt", Gt[0], [("Gt", 0, h) for h in range(4)])
                for bi in range(2):
                    kt = rot("ktok", 2)

                    def trk(e, par=par, bi=bi):
                        ins = None
                        for h in range(4):
                            ins = e.transpose(psb(7)[:, 512 + h * 128:512 + (h + 1) * 128],
                                              KtT[par][:, h, bi * 128:(bi + 1) * 128], ident[:, :])
                        return ins
                    add("pe", trk, reads=[("KtT", par, h) for h in range(4)] + ["ident"], writes=[("ps", 7)])
                    add("act", lambda e, kt=kt: e.activation(Ktok[kt], psb(7)[:, 512:1024], AF.Copy),
                        reads=[("ps", 7)], writes=[("Ktok", kt)])
                    for ce in range(2):
                        c = 2 * bi + ce
                        cg = 4 * tt + c
                        r0, r1 = ce * 64, ce * 64 + 64
                        tc0 = c * 64
                        ah = rot("A", 2)
                        at = rot("AT", 2)

                        def amm(e, par=par, tc0=tc0, r0=r0, r1=r1, ah=ah):
                            ins = None
                            for h in range(4):
                                ins = e.matmul(ps_t[r0:r1, 4, ah * 256 + h * 64:ah * 256 + (h + 1) * 64],
                                               KtT[par][:, h, tc0:tc0 + 64], QtT[par][:, h, tc0:tc0 + 64],
                                               start=True, stop=True)
                            return ins
                        add("pe", amm, reads=[("KtT", par, h) for h in range(4)] + [("QtT", par, h) for h in range(4)],
                            writes=[("ps", 4)])
                        add("dve", lambda e, r0=r0, r1=r1, ah=ah, at=at: e.tensor_tensor(
                            AT[at][r0:r1, :, :],
                            ps_t[r0:r1, 4, ah * 256:(ah + 1) * 256].rearrange("p (h t) -> p h t", h=4),
                            mask2[r0:r1, :].unsqueeze(1).to_broadcast([64, 4, 64]), ALU.mult),
                            reads=[("ps", 4), "mask2"], writes=[("AT", at, ce)])
                        so = cnt.get("S", 0) % 2
                        ob = (5, 2)[cg % 2]

                        def omm(e, par=par, bi=bi, tc0=tc0, r0=r0, r1=r1, at=at, cg=cg, so=so, ob=ob):
                            ins = None
                            for h in range(4):
                                ins = e.matmul(ps_t[r0:r1, ob, h * 128:(h + 1) * 128], AT[at][r0:r1, h, :],
                                               vtok[par][r0:r1, bi, h * 128:(h + 1) * 128],
                                               start=True, stop=(cg == 0))
                                if cg > 0:
                                    ins = e.matmul(ps_t[r0:r1, ob, h * 128:(h + 1) * 128],
                                                   QtT[par][:, h, tc0:tc0 + 64], Sbf[so][:, h, :],
                                                   start=False, stop=True)
                            return ins
                        add("pe", omm, reads=[("AT", at, ce), ("vtok", par, bi), ("Sbf", so)] +
                            [("QtT", par, h) for h in range(4)], writes=[("ps", ob)])
                        last = (cg == 31)
                        flush_now = list(dpend)
                        del dpend[:]
                        if not last:
                            def smm(e, par=par, bi=bi, r0=r0, r1=r1, kt=kt):
                                ins = None
                                for h in range(4):
                                    ins = e.matmul(ps(6)[:, h * 128:(h + 1) * 128],
                                                   Ktok[kt][r0:r1, h * 128:(h + 1) * 128],
                                                   vtok[par][r0:r1, bi, h * 128:(h + 1) * 128],
                                                   start=True, stop=True)
                                return ins
                            add("pe", smm, reads=[("Ktok", kt), ("vtok", par, bi)], writes=[("ps", 6)])
                            sn = 1 - so
                            cnt["S"] = cnt.get("S", 0) + 1
                            gb = gdec[par][:, :, c:c + 1].to_broadcast([128, 4, 128])
                            if cg == 0:
                                add("dve", lambda e, sn=sn, gb=gb: e.tensor_tensor(
                                    Sst[sn], ps(6).rearrange("p (h v) -> p h v", h=4), gb, ALU.mult),
                                    reads=[("ps", 6)] + [("gdec", par, h) for h in range(4)], writes=[("Sst", sn)])
                            else:
                                add("dve", lambda e, so=so: e.tensor_tensor(
                                    Ub, ps(6).rearrange("p (h v) -> p h v", h=4), Sst[so], ALU.add),
                                    reads=[("ps", 6), ("Sst", so)], writes=["Ub"])
                                add("pool", lambda e, sn=sn, gb=gb: e.tensor_tensor(Sst[sn], Ub, gb, ALU.mult),
                                    reads=["Ub"] + [("gdec", par, h) for h in range(4)], writes=[("Sst", sn)])
                            add("act", lambda e, sn=sn: e.activation(Sbf[sn], Sst[sn], AF.Copy),
                                reads=[("Sst", sn)], writes=[("Sbf", sn)])
                        for f_ in flush_now:
                            f_()
                        add("act", lambda e, r0=r0, r1=r1, ob=ob: e.activation(
                            osq[r0:r1, :, :], ps_t[r0:r1, ob, :].rearrange("p (h v) -> p h v", h=4), AF.Square),
                            reads=[("ps", ob)], writes=[("osq", ce)])
                        add("dve", lambda e, r0=r0, r1=r1: e.reduce_sum(ssD[r0:r1, 0, :], osq[r0:r1, :, :], AX.X),
                            reads=[("osq", ce)], writes=[("ssD", 0, ce)])
                        add("act", lambda e, r0=r0, r1=r1: e.activation(ssD[r0:r1, 1, :], ssD[r0:r1, 0, :], AF.Ln,
                                                                        bias=EPS, scale=1.0 / 128),
                            reads=[("ssD", 0, ce)], writes=[("ssD", 1, ce)])
                        add("act", lambda e, r0=r0, r1=r1: e.activation(ssD[r0:r1, 2, :], ssD[r0:r1, 1, :], AF.Exp, scale=-0.5),
                            reads=[("ssD", 1, ce)], writes=[("ssD", 2, ce)])
                        add("dve", lambda e, r0=r0, r1=r1, ob=ob: e.tensor_tensor(
                            onb[r0:r1, :, :], ps_t[r0:r1, ob, :].rearrange("p (h v) -> p h v", h=4),
                            ssD[r0:r1, 2, :].unsqueeze(2).to_broadcast([64, 4, 128]), ALU.mult),
                            reads=[("ps", ob), ("ssD", 2, ce)], writes=[("onb", ce)])
                        def d_tail(r0=r0, r1=r1, ce=ce, par=par, tok0=tok0, tc0=tc0, tt=tt, c=c):
                            ts_ = rot("tp7", 2)

                            def tro(e, r0=r0, r1=r1, ts_=ts_):
                                ins = None
                                for h in range(4):
                                    ins = e.transpose(psb(7)[:, ts_ * 256 + h * 64:ts_ * 256 + (h + 1) * 64],
                                                      onb[r0:r1, h, :], ident[r0:r1, r0:r1])
                                return ins
                            add("pe", tro, reads=[("onb", ce), "ident"], writes=[("ps", 7)])
                            add("dve", lambda e, ts_=ts_, par=par, tok0=tok0, tc0=tc0: e.scalar_tensor_tensor(
                                ocatT[:, 4:8, tok0 + tc0:tok0 + tc0 + 64],
                                psb(7)[:, ts_ * 256:(ts_ + 1) * 256].rearrange("p (h t) -> p h t", h=4),
                                hgw[:, 0:1], Gt[par][:, :, tc0:tc0 + 64], ALU.mult, ALU.mult),
                                reads=[("ps", 7), "hgw"] + [("Gt", par, h) for h in range(4)],
                                writes=[("ocatT", 4, tt, c)])
                        dpend.append(d_tail)
            for f_ in dpend:
                f_()
            del dpend[:]
            if b == 0:
                tap("ocatT", ocatT, [("ocatT", 4, tt, c) for tt in range(8) for c in range(4)] +
                    [("ocatT", h, u) for h in range(4) for u in range(8)])

            mark("D%d" % b)
            S.barrier()
            exs = [rot("xt", 3) for _ in range(NT)]

            def e_load(i, b=b, exs=exs):
                xs = exs[i]
                add("sp", lambda e, xs=xs, i=i, b=b: e.dma_start(out=xt[xs], in_=x[b, i * 128:(i + 1) * 128, :]),
                    writes=[("xt", xs)], chan=("xt", xs))
            e_load(0)
            e_load(1)
            for i in range(NT):
                xs = exs[i]
                pp = (0, 3)[rot("pp", 2)]

                def oproj(e, i=i, pp=pp):
                    ins = None
                    for half in range(2):
                        for kc in range(8):
                            ins = e.matmul(ps(pp + half), ocatT[:, kc, i * 128:(i + 1) * 128],
                                           wout[:, kc, half * 512:(half + 1) * 512],
                                           start=(kc == 0), stop=(kc == 7))
                    return ins
                add("pe", oproj, reads=["wout"], writes=[("ps", pp), ("ps", pp + 1)])
                x1s = rot("x1t", 2)
                add("dve", lambda e, xs=xs, x1s=x1s, pp=pp: e.tensor_tensor(
                    x1t[x1s].rearrange("p (a n) -> p a n", a=2), xt[xs].rearrange("p (a n) -> p a n", a=2),
                    ps_t[:, pp:pp + 2, :], ALU.add),
                    reads=[("xt", xs), ("ps", pp), ("ps", pp + 1)], writes=[("x1t", x1s)])
                si = rot("st", 8)
                ss, ln_, rs = stat(si)
                gi = b * NT + i
                add("act", lambda e, x1s=x1s, ss=ss: e.activation(junk, x1t[x1s], AF.Square, accum_out=ss),
                    reads=[("x1t", x1s)], writes=["junk", ("ss", si)])
                add("act", lambda e, ss=ss, ln_=ln_: e.activation(ln_, ss, AF.Ln, bias=EPS, scale=1.0 / D),
                    reads=[("ss", si)], writes=[("ln", si)])
                add("act", lambda e, gi=gi, ln_=ln_: e.activation(rstd2[:, gi:gi + 1], ln_, AF.Exp, scale=-0.5),
                    reads=[("ln", si)], writes=[("rstd2", gi)])
                if i + 2 < NT:
                    e_load(i + 2)
                add("sp", lambda e, x1s=x1s, i=i, b=b: e.dma_start(out=out[b, i * 128:(i + 1) * 128, :], in_=x1t[x1s]),
                    reads=[("x1t", x1s)], chan=("x1st", x1s))
                if b == 0 and i == 0:
                    tap("x1", x1t[x1s], [("x1t", x1s)])
            S.barrier()
            mark("E%d" % b)

        mark("P1")
        HC = NFC // 2
        WB = [0, 3, 11, 16, NFC]

        def wpiece(jc):
            for p_ in range(4):
                if WB[p_] <= jc < WB[p_ + 1]:
                    return p_

        def ld_wup(p_):
            c0, c1 = WB[p_] * 128, WB[p_ + 1] * 128
            for part in range(2):
                add("pool", lambda e, c0=c0, c1=c1, part=part: e.dma_start(
                    out=wup[:, :, part * FF + c0:part * FF + c1],
                    in_=w_up[:, part * FF + c0:part * FF + c1].rearrange("(k p) n -> p k n", p=128)),
                    writes=[("wup", p_, part)], chan=("wup", p_, part))

        def ld_wdown(hf_):
            add("pool", lambda e, hf_=hf_: e.dma_start(
                out=wdown[:, hf_ * HC:(hf_ + 1) * HC, :],
                in_=w_down[hf_ * HC * 128:(hf_ + 1) * HC * 128, :].rearrange("(c p) n -> p c n", p=128)),
                writes=[("wdown", hf_)], chan=("wdown", hf_))

        ld_wup(0); ld_wup(1); ld_wdown(0); ld_wup(2); ld_wup(3); ld_wdown(1)
        mark("W2")
        def p2_front(t):
            b, j = divmod(t, 8)
            xs = t % 2
            gi0 = b * NT + 2 * j
            add("sp", lambda e, xs=xs, j=j, b=b: e.dma_start(
                out=x1p[xs], in_=out[b, j * 256:(j + 1) * 256, :].rearrange("(s p) d -> p s d", p=128)),
                writes=[("x1p", xs)], chan=("x1ld", xs))
            for s_ in range(2):
                add("dve", lambda e, xs=xs, s_=s_, gi0=gi0: e.tensor_scalar(
                    h2n[xs][:, s_, :], x1p[xs][:, s_, :], rstd2[:, gi0 + s_:gi0 + s_ + 1], None, ALU.mult),
                    reads=[("x1p", xs)], writes=[("h2n", xs, s_)])
            hx = xs
            for kh in range(2):
                tb = 2

                def trh(e, xs=xs, kh=kh, tb=tb):
                    ins = None
                    for kq in range(4):
                        kc = kh * 4 + kq
                        for s_ in range(2):
                            ins = e.transpose(psb(tb)[:, kq * 256 + s_ * 128:kq * 256 + (s_ + 1) * 128],
                                              h2n[xs][:, s_, kc * 128:(kc + 1) * 128], ident[:, :])
                    return ins
                add("pe", trh, reads=[("h2n", xs, 0), ("h2n", xs, 1)], writes=[("ps", tb)])
                add("dve", lambda e, hx=hx, kh=kh, tb=tb: e.tensor_tensor(
                    h2T[hx][:, kh * 4:(kh + 1) * 4, :],
                    psb(tb)[:, 0:1024].rearrange("p (k t) -> p k t", k=4),
                    ln2c[:, kh * 4:(kh + 1) * 4].unsqueeze(2).to_broadcast([128, 4, 256]), ALU.mult),
                    reads=[("ps", tb)], writes=[("h2T", hx, kh)])

        p2_front(0)
        for b in range(NB):
            for j in range(8):
                t_ = b * 8 + j
                xs = t_ % 2
                hx = xs
                gk_of = {}

                def emit_down(jc, gk_of=gk_of):
                    gk = gk_of[jc]

                    def dmm(e, jc=jc, gk=gk):
                        ins = None
                        for s in range(2):
                            for half in range(2):
                                ins = e.matmul(ps(4 + 2 * s + half), gbuf[gk][:, s * 128:(s + 1) * 128],
                                               wdown[:, jc, half * 512:(half + 1) * 512],
                                               start=(jc == 0), stop=(jc == NFC - 1))
                        return ins
                    add("pe", dmm, reads=[("g", gk), ("wdown", jc // HC)], writes=[("ps", 4), ("ps", 5), ("ps", 6), ("ps", 7)])

                for jc in range(NFC):
                    ub = (0, 1, 3)[rot("uv", 3)]

                    def upmm(e, jc=jc, ub=ub, hx=hx):
                        ins = None
                        for part in range(2):
                            for kc in range(8):
                                ins = e.matmul(ps(ub)[:, part * 256:(part + 1) * 256],
                                               wup[:, kc, part * FF + jc * 128:part * FF + (jc + 1) * 128],
                                               h2T[hx][:, kc, :], start=(kc == 0), stop=(kc == 7))
                        return ins
                    add("pe", upmm, reads=[("wup", wpiece(jc), 0), ("wup", wpiece(jc), 1), ("h2T", hx, 0), ("h2T", hx, 1)],
                        writes=[("ps", ub)])
                    ai = rot("acc2", 3)
                    a = accb[ai]
                    add("act", lambda e, a=a, ub=ub, jc=jc: e.activation(
                        a, ps(ub)[:, 0:256], AF.Identity, scale=cw[:, 2, jc:jc + 1], bias=cb[:, jc:jc + 1]),
                        reads=[("ps", ub)], writes=[("acc2", ai)])
                    add("dve", lambda e, a=a, ub=ub, jc=jc: e.scalar_tensor_tensor(
                        a[:, 1:256], ps(ub)[:, 0:255], cw[:, 1, jc:jc + 1], a[:, 1:256], ALU.mult, ALU.add),
                        reads=[("ps", ub), ("acc2", ai)], writes=[("acc2", ai)])
                    add("dve", lambda e, a=a, ub=ub, jc=jc: e.scalar_tensor_tensor(
                        a[:, 2:256], ps(ub)[:, 0:254], cw[:, 0, jc:jc + 1], a[:, 2:256], ALU.mult, ALU.add),
                        reads=[("ps", ub), ("acc2", ai)], writes=[("acc2", ai)])
                    if j > 0:
                        add("dve", lambda e, a=a, jc=jc: e.scalar_tensor_tensor(
                            a[:, 0:1], halo[:, jc, 1:2], cw[:, 1, jc:jc + 1], a[:, 0:1], ALU.mult, ALU.add),
                            reads=[("halo", jc), ("acc2", ai)], writes=[("acc2", ai)])
                        add("dve", lambda e, a=a, jc=jc: e.scalar_tensor_tensor(
                            a[:, 0:2], halo[:, jc, 0:2], cw[:, 0, jc:jc + 1], a[:, 0:2], ALU.mult, ALU.add),
                            reads=[("halo", jc), ("acc2", ai)], writes=[("acc2", ai)])
                    add("act", lambda e, ub=ub, jc=jc: e.activation(halo[:, jc, :], ps(ub)[:, 254:256], AF.Copy),
                        reads=[("ps", ub)], writes=[("halo", jc)])
                    sk = rot("sl", 2)
                    add("act", lambda e, a=a, sk=sk: e.activation(slb[sk], a, AF.Silu),
                        reads=[("acc2", ai)], writes=[("sl", sk)])
                    gk = rot("g", 4)
                    gk_of[jc] = gk
                    add("dve", lambda e, sk=sk, gk=gk, ub=ub: e.tensor_tensor(
                        gbuf[gk], slb[sk], ps(ub)[:, 256:512], ALU.mult),
                        reads=[("sl", sk), ("ps", ub)], writes=[("g", gk)])
                    if jc >= 2:
                        emit_down(jc - 2)
                    if jc == 9 and t_ + 1 < NB * 8:
                        p2_front(t_ + 1)
                emit_down(NFC - 2)
                emit_down(NFC - 1)
                for s in range(2):
                    add("dve", lambda e, xs=xs, s=s: e.tensor_tensor(
                        x1p[xs][:, s, :].rearrange("p (a n) -> p a n", a=2),
                        x1p[xs][:, s, :].rearrange("p (a n) -> p a n", a=2),
                        ps_t[:, 4 + 2 * s:6 + 2 * s, :], ALU.add),
                        reads=[("x1p", xs), ("ps", 4 + 2 * s), ("ps", 5 + 2 * s)], writes=[("x1p", xs)])
                add("sp", lambda e, xs=xs, j=j, b=b: e.dma_start(
                    out=out[b, j * 256:(j + 1) * 256, :].rearrange("(s p) d -> p s d", p=128), in_=x1p[xs]),
                    reads=[("x1p", xs)], chan=("ost", xs))
        mark("END")
        lim = None
        if limit_mark is not None:
            lim = marks[limit_mark]
        if marks_out is not None:
            marks_out.update(marks)
        S.emit(limit=lim)
    return nc


_PARAM_NAMES = ["ln1_w", "w_in", "q_norm_w", "k_norm_w", "lam_q1", "lam_k1", "lam_q2", "lam_k2",
                "diff_subln_w", "hgrn_lb_logits", "hgrn_norm_w", "w_out", "ln2_w", "w_up", "conv_w",
                "conv_b", "w_down"]


def _core_inputs(inputs, core):
    m = {"x": np.ascontiguousarray(inputs["x"][core * NB:(core + 1) * NB], dtype=np.float32)}
    for n in _PARAM_NAMES:
        a = np.asarray(inputs[n], dtype=np.float32)
        if n == "hgrn_lb_logits":
            m[n] = np.ascontiguousarray(a)
        elif n in ("w_in", "w_out", "w_up", "w_down", "conv_w"):
            m[n] = np.ascontiguousarray(a[0])
        else:
            m[n] = np.ascontiguousarray(a.reshape(1, -1))
    return m


def kernel(**inputs):
    nc = build_program()
    in_maps = [_core_inputs(inputs, c) for c in range(NCORES)]
    res = run_bass_kernel_spmd(nc, in_maps, core_ids=list(range(NCORES)))
    outs = [np.asarray(r["out"], dtype=np.float32) for r in res.results]
    return np.concatenate(outs, axis=0)
```
